# Optimizing a Trainium2 kernel written in Bass

```python
import math
import numpy as np
import jax
import jax.numpy as jnp
from jax import lax

D_MODEL = 4096
BATCH = 8
SEQ = 2048
DEPTH = 2

CTX_LEN = 256
GRID_W = 64
HEAD_DIM = 128
MIX_W = D_MODEL
N_MIX_HEADS = MIX_W // HEAD_DIM
GDN_HEADS = 3 * N_MIX_HEADS // 8
ATT_HEADS = 3 * N_MIX_HEADS // 8
ATT_KV_HEADS = ATT_HEADS // 3
HGRN_HEADS = N_MIX_HEADS - GDN_HEADS - ATT_HEADS
GDN_W = GDN_HEADS * HEAD_DIM
ATT_W = ATT_HEADS * HEAD_DIM
ATT_KV_W = ATT_KV_HEADS * HEAD_DIM
HGRN_W = HGRN_HEADS * HEAD_DIM
CONV_W = 5
CHUNK = 64
Q_BLOCK = 128
ROPE_THETA = 10000.0
N_GROUPS = 4
EXPERTS_PER_GROUP = 8
N_EXPERTS = N_GROUPS * EXPERTS_PER_GROUP
TOP_K = 2
EXPERT_FF = 512
ADA_CHUNKS = 6
EPS = 1e-6

IN_SIZES = (3 * GDN_W, GDN_W, 2 * GDN_HEADS, 2 * GDN_HEADS, ATT_W, ATT_KV_W, ATT_KV_W,
            HGRN_W, HGRN_W, HGRN_W, HGRN_W, HGRN_W)
IN_OFFSETS = tuple(int(o) for o in np.cumsum(IN_SIZES)[:-1])
N_IN = sum(IN_SIZES)

kernel_name = 'hybrid_gdn_gqa_hgrn2_hmoe_prefix_dit'


def rmsnorm(x, g):
    xf = x.astype(jnp.float32)
    y = xf * lax.rsqrt(jnp.mean(xf * xf, axis=-1, keepdims=True) + EPS)
    return (y * g.astype(jnp.float32)).astype(x.dtype)


def l2norm(x):
    return x * lax.rsqrt(jnp.sum(x * x, axis=-1, keepdims=True) + EPS)


def short_conv(u, w):
    pad = CONV_W // 2
    return lax.conv_general_dilated(u, w[:, None, :].astype(u.dtype), window_strides=(1,),
                                    padding=((pad, pad),), dimension_numbers=('NWC', 'WIO', 'NWC'),
                                    feature_group_count=u.shape[-1])


def _split_heads(t, n_heads):
    return t.reshape(t.shape[:2] + (n_heads, HEAD_DIM))


def _chunk(t):
    b, l, h = t.shape[:3]
    t = t.reshape((b, l // CHUNK, CHUNK, h) + t.shape[3:])
    return jnp.moveaxis(t, (1, 2), (0, 3))


def _unchunk(t):
    t = jnp.moveaxis(t, (0, 3), (1, 2))
    return t.reshape((t.shape[0], t.shape[1] * t.shape[2]) + t.shape[3:])


def _rotate_half_axial(x):
    def rh(v):
        a, b = jnp.split(v, 2, axis=-1)
        return jnp.concatenate([-b, a], axis=-1)
    xr, xc = jnp.split(x, 2, axis=-1)
    return jnp.concatenate([rh(xr), rh(xc)], axis=-1)


def axial_rope_tables(n_tokens):
    rows = n_tokens // GRID_W
    row = jnp.repeat(jnp.arange(rows, dtype=jnp.float32), GRID_W)
    col = jnp.tile(jnp.arange(GRID_W, dtype=jnp.float32), rows)
    half = HEAD_DIM // 2
    inv_freq = ROPE_THETA ** (-jnp.arange(0, half, 2, dtype=jnp.float32) / half)
    ang_r = row[:, None] * inv_freq[None, :]
    ang_c = col[:, None] * inv_freq[None, :]
    ang = jnp.concatenate([ang_r, ang_r, ang_c, ang_c], axis=-1)
    return jnp.cos(ang)[:, None, :], jnp.sin(ang)[:, None, :]


def apply_rope(x, cos, sin):
    xf = x.astype(jnp.float32)
    return (xf * cos + _rotate_half_axial(xf) * sin).astype(x.dtype)


def _bidir(scan_fn, ctx_f, lat_f, ctx_b, lat_b, s0):
    flip = lambda ts: tuple(jnp.flip(t, axis=1) for t in ts)
    o_cf, s_cf = scan_fn(*ctx_f, s0)
    o_lf, _ = scan_fn(*lat_f, s_cf)
    o_cb, s_cb = scan_fn(*flip(ctx_b), s0)
    o_lb, _ = scan_fn(*flip(lat_b), s_cb)
    return o_cf + jnp.flip(o_cb, axis=1), o_lf + jnp.flip(o_lb, axis=1)


def gated_delta_scan(q, k, v, g, beta, s0):
    dv = v.shape[-1]
    qc, kc, vc = _chunk(q), _chunk(k), _chunk(v)
    gc = jnp.cumsum(_chunk(g), axis=-1)
    bc = _chunk(beta)
    idx = jnp.arange(CHUNK)
    incl = idx[:, None] >= idx[None, :]
    strict = idx[:, None] > idx[None, :]
    decay_incl = jnp.exp(jnp.where(incl, gc[..., :, None] - gc[..., None, :], -jnp.inf))
    decay_strict = jnp.where(strict, decay_incl, 0.0)
    kb = kc * bc[..., None]
    a_kk = jnp.einsum('nbhid,nbhjd->nbhij', kb, kc) * decay_strict
    eye = jnp.eye(CHUNK, dtype=a_kk.dtype)
    rhs = jnp.concatenate([vc * bc[..., None], kb * jnp.exp(gc)[..., None]], axis=-1)
    sol = lax.linalg.triangular_solve(eye + a_kk, rhs, left_side=True, lower=True)
    u, w = sol[..., :dv], sol[..., dv:]
    a_qk = jnp.einsum('nbhid,nbhjd->nbhij', qc, kc) * decay_incl

    def step(s, inp):
        q_i, k_i, u_i, w_i, a_i, g_i = inp
        v_new = u_i - jnp.einsum('bhck,bhkv->bhcv', w_i, s)
        o = (jnp.einsum('bhck,bhkv->bhcv', q_i * jnp.exp(g_i)[..., None], s)
             + jnp.einsum('bhij,bhjv->bhiv', a_i, v_new))
        g_last = g_i[..., -1:]
        s = (jnp.exp(g_last)[..., None] * s
             + jnp.einsum('bhck,bhcv->bhkv', k_i * jnp.exp(g_last - g_i)[..., None], v_new))
        return s, o

    s_fin, o = lax.scan(step, s0, (qc, kc, u, w, a_qk, gc))
    return _unchunk(o), s_fin


def gla_scan(q, k, v, log_f, s0):
    qc, kc, vc = _chunk(q), _chunk(k), _chunk(v)
    gc = jnp.cumsum(_chunk(log_f), axis=-2)
    idx = jnp.arange(CHUNK)
    incl = (idx[:, None] >= idx[None, :])[:, :, None]

    def step(s, inp):
        q_i, k_i, v_i, g_i = inp
        dec = jnp.exp(jnp.where(incl, g_i[:, :, :, None, :] - g_i[:, :, None, :, :], -jnp.inf))
        a = jnp.einsum('bhid,bhjd,bhijd->bhij', q_i, k_i, dec)
        o = (jnp.einsum('bhck,bhkv->bhcv', q_i * jnp.exp(g_i), s)
             + jnp.einsum('bhij,bhjv->bhiv', a, v_i))
        g_last = g_i[:, :, -1:, :]
        s = (jnp.exp(g_last[:, :, 0, :])[..., None] * s
             + jnp.einsum('bhck,bhcv->bhkv', k_i * jnp.exp(g_last - g_i), v_i))
        return s, o

    s_fin, o = lax.scan(step, s0, (qc, kc, vc, gc))
    return _unchunk(o), s_fin


def _gated_head_norm(o, z, g):
    bsz, n = z.shape[:2]
    zh = z.reshape(bsz, n, -1, HEAD_DIM).astype(jnp.float32)
    return (rmsnorm(o, g) * jax.nn.silu(zh)).reshape(bsz, n, -1).astype(z.dtype)


def _gdn_prep(qkv, a, b, conv_w, a_log, dt_bias):
    qkv = jax.nn.silu(short_conv(qkv, conv_w)).astype(jnp.float32)
    q, k, v = jnp.split(qkv, 3, axis=-1)
    q = l2norm(_split_heads(q, GDN_HEADS)) * HEAD_DIM ** -0.5
    k = l2norm(_split_heads(k, GDN_HEADS))
    v = _split_heads(v, GDN_HEADS)
    bsz, n = a.shape[:2]
    a = a.astype(jnp.float32).reshape(bsz, n, 2, GDN_HEADS)
    beta = jax.nn.sigmoid(b.astype(jnp.float32).reshape(bsz, n, 2, GDN_HEADS))
    g = -jnp.exp(a_log.astype(jnp.float32)) * jax.nn.softplus(a + dt_bias.astype(jnp.float32))
    return (q, k, v, g[:, :, 0], beta[:, :, 0]), (q, k, v, g[:, :, 1], beta[:, :, 1])


def gdn_mixer(ctx_in, lat_in, conv_w, a_log, dt_bias, norm_g, need_ctx):
    ctx_f, ctx_b = _gdn_prep(ctx_in[0], ctx_in[2], ctx_in[3], conv_w, a_log, dt_bias)
    lat_f, lat_b = _gdn_prep(lat_in[0], lat_in[2], lat_in[3], conv_w, a_log, dt_bias)
    s0 = jnp.zeros((ctx_in[0].shape[0], GDN_HEADS, HEAD_DIM, HEAD_DIM), jnp.float32)
    o_c, o_l = _bidir(gated_delta_scan, ctx_f, lat_f, ctx_b, lat_b, s0)
    y_l = _gated_head_norm(o_l, lat_in[1], norm_g)
    y_c = _gated_head_norm(o_c, ctx_in[1], norm_g) if need_ctx else None
    return y_c, y_l


def _gqa_attend(q, k, v):
    bsz, lq = q.shape[:2]
    qg = q.reshape(bsz, lq, ATT_KV_HEADS, ATT_HEADS // ATT_KV_HEADS, HEAD_DIM)
    s = jnp.einsum('bqkgd,bskd->bkgqs', qg, k, preferred_element_type=jnp.float32) * (HEAD_DIM ** -0.5)
    p = jax.nn.softmax(s, axis=-1).astype(v.dtype)
    o = jnp.einsum('bkgqs,bskd->bqkgd', p, v)
    return o.reshape(bsz, lq, ATT_W)


def attention_mixer(ctx_in, lat_in, q_norm_g, k_norm_g, cos, sin, need_ctx):
    q_c, k_c, v_c = ctx_in
    q_l, k_l, v_l = lat_in
    k_c = rmsnorm(_split_heads(k_c, ATT_KV_HEADS), k_norm_g)
    k_l = apply_rope(rmsnorm(_split_heads(k_l, ATT_KV_HEADS), k_norm_g), cos, sin)
    q_l = apply_rope(rmsnorm(_split_heads(q_l, ATT_HEADS), q_norm_g), cos, sin)
    v_c = _split_heads(v_c, ATT_KV_HEADS)
    v_l = _split_heads(v_l, ATT_KV_HEADS)
    k_all = jnp.concatenate([k_c, k_l], axis=1)
    v_all = jnp.concatenate([v_c, v_l], axis=1)
    bsz, n_lat = q_l.shape[:2]
    q_blocks = jnp.moveaxis(q_l.reshape(bsz, n_lat // Q_BLOCK, Q_BLOCK, ATT_HEADS, HEAD_DIM), 1, 0)
    y_l = lax.map(lambda qb: _gqa_attend(qb, k_all, v_all), q_blocks)
    y_l = jnp.moveaxis(y_l, 0, 1).reshape(bsz, n_lat, ATT_W)
    y_c = _gqa_attend(rmsnorm(_split_heads(q_c, ATT_HEADS), q_norm_g), k_c, v_c) if need_ctx else None
    return y_c, y_l


def _hgrn_prep(q, f_fwd, f_bwd, i, lb):
    qh = jax.nn.silu(_split_heads(q, HGRN_HEADS).astype(jnp.float32)) * HEAD_DIM ** -0.5
    vh = _split_heads(i, HGRN_HEADS).astype(jnp.float32)
    lbh = lb.astype(jnp.float32).reshape(HGRN_HEADS, HEAD_DIM)

    def gate(f):
        z = _split_heads(f, HGRN_HEADS).astype(jnp.float32)
        log_f = jnp.logaddexp(jax.nn.log_sigmoid(z), jnp.log(lbh) + jax.nn.log_sigmoid(-z))
        return (1.0 - lbh) * jax.nn.sigmoid(-z), log_f

    k_f, lf_f = gate(f_fwd)
    k_b, lf_b = gate(f_bwd)
    return (qh, k_f, vh, lf_f), (qh, k_b, vh, lf_b)


def hgrn_mixer(ctx_in, lat_in, lb, norm_g, need_ctx):
    ctx_f, ctx_b = _hgrn_prep(ctx_in[0], ctx_in[1], ctx_in[2], ctx_in[3], lb)
    lat_f, lat_b = _hgrn_prep(lat_in[0], lat_in[1], lat_in[2], lat_in[3], lb)
    s0 = jnp.zeros((ctx_in[0].shape[0], HGRN_HEADS, HEAD_DIM, HEAD_DIM), jnp.float32)
    o_c, o_l = _bidir(gla_scan, ctx_f, lat_f, ctx_b, lat_b, s0)
    y_l = _gated_head_norm(o_l, lat_in[4], norm_g)
    y_c = _gated_head_norm(o_c, ctx_in[4], norm_g) if need_ctx else None
    return y_c, y_l


def hier_moe(h, rgw, rgb, rew, reb, w_gate, w_up, w_down):
    shp = h.shape
    t = h.reshape(-1, shp[-1])
    n_tok = t.shape[0]
    grp_logits = (t @ rgw + rgb).astype(jnp.float32)
    p_grp = jax.nn.softmax(grp_logits, axis=-1)
    grp = jnp.argmax(grp_logits, axis=-1)
    exp_logits = (t @ rew + reb).astype(jnp.float32).reshape(n_tok, N_GROUPS, EXPERTS_PER_GROUP)
    tok = jnp.arange(n_tok)
    p_in = jax.nn.softmax(exp_logits[tok, grp], axis=-1)
    top_p, top_i = lax.top_k(p_in, TOP_K)
    w = p_grp[tok, grp][:, None] * top_p / jnp.sum(top_p, axis=-1, keepdims=True)
    eid = grp[:, None] * EXPERTS_PER_GROUP + top_i
    combine = jnp.sum(jax.nn.one_hot(eid, N_EXPERTS, dtype=jnp.float32) * w[..., None], axis=1).astype(t.dtype)
    y = jnp.zeros_like(t)
    for e in range(N_EXPERTS):
        he = jax.nn.silu(t @ w_gate[e]) * (t @ w_up[e])
        y = y + combine[:, e:e + 1] * (he @ w_down[e])
    return y.reshape(shp)


def _layer(x_lat, x_ctx, silu_c, silu_cc, cos, sin, lb, p, need_ctx):
    mod_l = jnp.split((silu_c @ p['ada_w'] + p['ada_b'])[:, None, :], ADA_CHUNKS, axis=-1)
    mod_c = jnp.split(silu_cc @ p['ada_w'] + p['ada_b'], ADA_CHUNKS, axis=-1)
    n_ctx = x_ctx.shape[1]

    h_c = rmsnorm(x_ctx, p['norm1_g']) * (1 + mod_c[1]) + mod_c[0]
    h_l = rmsnorm(x_lat, p['norm1_g']) * (1 + mod_l[1]) + mod_l[0]
    proj = jnp.concatenate([h_c, h_l], axis=1) @ p['w_in']
    groups = jnp.split(proj, IN_OFFSETS, axis=-1)
    cg = [gr[:, :n_ctx] for gr in groups]
    lg = [gr[:, n_ctx:] for gr in groups]
    gdn_c, gdn_l = gdn_mixer(cg[0:4], lg[0:4], p['gdn_conv_w'], p['gdn_a_log'], p['gdn_dt_bias'],
                             p['gdn_norm_g'], need_ctx)
    att_c, att_l = attention_mixer(cg[4:7], lg[4:7], p['attn_q_norm_g'], p['attn_k_norm_g'], cos, sin, need_ctx)
    hg_c, hg_l = hgrn_mixer(cg[7:12], lg[7:12], lb, p['hgrn_norm_g'], need_ctx)
    x_lat = x_lat + mod_l[2] * (jnp.concatenate([gdn_l, att_l, hg_l], axis=-1) @ p['w_out'])

    moe_args = (p['router_group_w'], p['router_group_b'], p['router_expert_w'], p['router_expert_b'],
                p['moe_w_gate'], p['moe_w_up'], p['moe_w_down'])
    h2_l = rmsnorm(x_lat, p['norm2_g']) * (1 + mod_l[4]) + mod_l[3]
    if need_ctx:
        x_ctx = x_ctx + mod_c[2] * (jnp.concatenate([gdn_c, att_c, hg_c], axis=-1) @ p['w_out'])
        h2_c = rmsnorm(x_ctx, p['norm2_g']) * (1 + mod_c[4]) + mod_c[3]
        y = hier_moe(jnp.concatenate([h2_c, h2_l], axis=1), *moe_args)
        x_ctx = x_ctx + mod_c[5] * y[:, :n_ctx]
        x_lat = x_lat + mod_l[5] * y[:, n_ctx:]
    else:
        x_lat = x_lat + mod_l[5] * hier_moe(h2_l, *moe_args)
    return x_lat, x_ctx


def setup_inputs(seed: int = 0) -> dict:
    key = jax.random.key(seed)
    ks = jax.random.split(key, 27)
    nrm = jax.random.normal
    d = D_MODEL
    f32 = jnp.float32

    def gain(k, shape):
        return 1.0 + 0.02 * nrm(k, shape, f32)

    dt = jnp.exp(jax.random.uniform(ks[9], (DEPTH, 2, GDN_HEADS), f32, math.log(1e-3), math.log(1e-1)))
    return {
        'x': nrm(ks[0], (BATCH, SEQ, d), f32),
        'c': nrm(ks[1], (BATCH, d), f32),
        'ctx': nrm(ks[2], (BATCH, CTX_LEN, d), f32),
        'c_ctx': nrm(ks[3], (d,), f32),
        'ada_w': nrm(ks[4], (DEPTH, d, ADA_CHUNKS * d), f32) * (0.5 * d ** -0.5),
        'ada_b': 0.02 * nrm(ks[5], (DEPTH, ADA_CHUNKS * d), f32),
        'norm1_g': gain(ks[6], (DEPTH, d)),
        'norm2_g': gain(ks[7], (DEPTH, d)),
        'w_in': nrm(ks[8], (DEPTH, d, N_IN), f32) * d ** -0.5,
        'gdn_conv_w': nrm(ks[10], (DEPTH, CONV_W, 3 * GDN_W), f32) * CONV_W ** -0.5,
        'gdn_a_log': jnp.log(jax.random.uniform(ks[11], (DEPTH, 2, GDN_HEADS), f32, 1.0, 16.0)),
        'gdn_dt_bias': dt + jnp.log(-jnp.expm1(-dt)),
        'gdn_norm_g': gain(ks[12], (DEPTH, HEAD_DIM)),
        'attn_q_norm_g': gain(ks[13], (DEPTH, HEAD_DIM)),
        'attn_k_norm_g': gain(ks[14], (DEPTH, HEAD_DIM)),
        'hgrn_lb_logits': 0.5 * nrm(ks[15], (DEPTH, HGRN_W), f32),
        'hgrn_norm_g': gain(ks[16], (DEPTH, HEAD_DIM)),
        'w_out': nrm(ks[17], (DEPTH, MIX_W, d), f32) * MIX_W ** -0.5,
        'router_group_w': nrm(ks[18], (DEPTH, d, N_GROUPS), f32) * d ** -0.5,
        'router_group_b': 0.01 * nrm(ks[19], (DEPTH, N_GROUPS), f32),
        'router_expert_w': nrm(ks[20], (DEPTH, d, N_EXPERTS), f32) * d ** -0.5,
        'router_expert_b': 0.01 * nrm(ks[21], (DEPTH, N_EXPERTS), f32),
        'moe_w_gate': nrm(ks[22], (DEPTH, N_EXPERTS, d, EXPERT_FF), f32) * d ** -0.5,
        'moe_w_up': nrm(ks[23], (DEPTH, N_EXPERTS, d, EXPERT_FF), f32) * d ** -0.5,
        'moe_w_down': nrm(ks[24], (DEPTH, N_EXPERTS, EXPERT_FF, d), f32) * EXPERT_FF ** -0.5,
        'final_norm_g': gain(ks[26], (d,)),
    }


def reference(x, c, ctx, c_ctx, ada_w, ada_b, norm1_g, norm2_g, w_in, gdn_conv_w, gdn_a_log, gdn_dt_bias,
              gdn_norm_g, attn_q_norm_g, attn_k_norm_g, hgrn_lb_logits, hgrn_norm_g, w_out,
              router_group_w, router_group_b, router_expert_w, router_expert_b,
              moe_w_gate, moe_w_up, moe_w_down, final_norm_g):
    cos, sin = axial_rope_tables(x.shape[1])
    lb_p = jax.nn.softmax(hgrn_lb_logits.astype(jnp.float32), axis=0)
    lb_cum = jnp.cumsum(lb_p, axis=0)
    lb_all = lb_cum - lb_cum[0:1]
    silu_c = jax.nn.silu(c)
    silu_cc = jax.nn.silu(c_ctx)
    x_lat, x_ctx = x, ctx
    for l in range(DEPTH):
        p = {
            'ada_w': ada_w[l], 'ada_b': ada_b[l], 'norm1_g': norm1_g[l], 'norm2_g': norm2_g[l],
            'w_in': w_in[l], 'gdn_conv_w': gdn_conv_w[l], 'gdn_a_log': gdn_a_log[l],
            'gdn_dt_bias': gdn_dt_bias[l], 'gdn_norm_g': gdn_norm_g[l],
            'attn_q_norm_g': attn_q_norm_g[l], 'attn_k_norm_g': attn_k_norm_g[l],
            'hgrn_norm_g': hgrn_norm_g[l], 'w_out': w_out[l],
            'router_group_w': router_group_w[l], 'router_group_b': router_group_b[l],
            'router_expert_w': router_expert_w[l], 'router_expert_b': router_expert_b[l],
            'moe_w_gate': moe_w_gate[l], 'moe_w_up': moe_w_up[l], 'moe_w_down': moe_w_down[l],
        }
        x_lat, x_ctx = _layer(x_lat, x_ctx, silu_c, silu_cc, cos, sin, lb_all[l], p,
                              need_ctx=(l < DEPTH - 1))
    return rmsnorm(x_lat, final_norm_g)
```

```python
import contextlib
import numpy as np
import ml_dtypes
import concourse.bass as bass
import concourse.mybir as mybir
from concourse.bass_utils import run_bass_kernel_spmd

F32 = mybir.dt.float32
BF16 = mybir.dt.bfloat16
I32 = mybir.dt.int32
AF = mybir.ActivationFunctionType
ALU = mybir.AluOpType
AX = mybir.AxisListType

SEM_LIMIT = 30000
DBG = {}
DMA_RING = 8


class V:
    __slots__ = ("ap", "keys")

    def __init__(self, ap, keys):
        self.ap = ap
        self.keys = keys if isinstance(keys, tuple) else (keys,)


class Tl:
    def __init__(self, handle, key, psum=False):
        self.h = handle
        self.key = key
        self.psum = psum

    def __getitem__(self, idx):
        return V(self.h[idx], (self.key,))

    def sub(self, sk):
        return self if self.psum else _Sub(self, sk)

    def v(self, ap, sk=None):
        return V(ap, ((self.key, sk),) if sk is not None else (self.key,))


class _Sub:
    def __init__(self, tl, sk):
        self.tl = tl
        self.sk = sk

    def __getitem__(self, idx):
        return V(self.tl.h[idx], ((self.tl.key, self.sk),))


class KB:
    def __init__(self, nc):
        self.nc = nc
        self.eng = {"pe": nc.tensor, "dve": nc.vector, "act": nc.scalar, "pool": nc.gpsimd, "sp": nc.sync}
        self.sem = {}
        self.cnt = {}
        self.nsem = 0
        self.all_sems = []
        for e in self.eng:
            self._new_sem(e)
        self.dma_ring = {}
        self.dma_n = {}
        self.seen = {e: {} for e in self.eng}
        self.last_w = {}
        self.readers = {}
        self.stack = contextlib.ExitStack()
        self.uid = 0
        self.n_instr = 0

    def _new_sem(self, e):
        self.nsem += 1
        s = self.nc.alloc_semaphore(f"s_{e}_{self.nsem}")
        self.all_sems.append(s)
        self.sem[e] = s
        self.cnt[e] = 0

    def _wait(self, e, ev):
        sem, val, sid = ev[0], ev[1], ev[2]
        if self.seen[e].get(sid, 0) >= val:
            return
        self.eng[e].wait_ge(sem, val)
        self.seen[e][sid] = val
        self.n_instr += 1

    def _deps(self, e, reads, writes):
        evs = {}

        def add(ev):
            if ev is None:
                return
            sid = ev[2]
            if sid not in evs or evs[sid][1] < ev[1]:
                evs[sid] = ev
        for k in reads:
            add(self.last_w.get(k))
        for k in writes:
            add(self.last_w.get(k))
            for ev in self.readers.get(k, {}).values():
                add(ev)
        for ev in evs.values():
            if e == "pe" and ev[3] == "pe":
                continue
            self._wait(e, ev)

    def _record(self, ev, reads, writes):
        for k in writes:
            self.last_w[k] = ev
            self.readers[k] = {}
        for k in reads:
            self.readers.setdefault(k, {})[ev[2]] = ev

    @staticmethod
    def _keys(vs):
        ks = []
        for v in vs:
            if isinstance(v, V):
                ks.extend(v.keys)
        return ks

    def issue(self, e, fn, reads, writes):
        reads = self._keys(reads)
        writes = self._keys(writes)
        if self.cnt[e] >= SEM_LIMIT:
            self._new_sem(e)
        self._deps(e, reads, writes)
        ins = fn()
        self.cnt[e] += 1
        ins.then_inc(self.sem[e], 1)
        ev = (self.sem[e], self.cnt[e], id(self.sem[e]), e)
        self._record(ev, reads, writes)
        self.n_instr += 1
        return ev

    def dma(self, q, out, in_, extra_r=(), extra_w=(), fn=None, **kw):
        reads = self._keys([in_, *extra_r])
        writes = self._keys([out, *extra_w])
        ring = self.dma_ring.setdefault(q, [])
        n = self.dma_n.get(q, 0)
        self.dma_n[q] = n + 1
        slot = n % DMA_RING
        if len(ring) <= slot:
            self.nsem += 1
            ring.append([self.nc.alloc_semaphore(f"d_{q}_{self.nsem}"), 0])
            self.all_sems.append(ring[-1][0])
        ent = ring[slot]
        if ent[1] * 16 >= SEM_LIMIT:
            self._wait(q, (ent[0], ent[1] * 16, id(ent[0]), "dma"))
            self.nsem += 1
            ent[0] = self.nc.alloc_semaphore(f"d_{q}_{self.nsem}")
            self.all_sems.append(ent[0])
            ent[1] = 0
        if ent[1] > 0:
            self._wait(q, (ent[0], ent[1] * 16, id(ent[0]), "dma"))
        self._deps(q, reads, writes)
        ins = fn() if fn is not None else self.eng[q].dma_start(out=out.ap, in_=in_.ap, **kw)
        ent[1] += 1
        ins.then_inc(ent[0], 16)
        ev = (ent[0], ent[1] * 16, id(ent[0]), "dma")
        self._record(ev, reads, writes)
        self.n_instr += 1
        return ev

    def barrier(self):
        evs = []
        for e in self.eng:
            if self.cnt[e] > 0:
                evs.append((self.sem[e], self.cnt[e], id(self.sem[e]), e))
        for q, ring in self.dma_ring.items():
            for ent in ring:
                if ent[1] > 0:
                    evs.append((ent[0], ent[1] * 16, id(ent[0]), "dma"))
        for e in self.eng:
            for ev in evs:
                if ev[3] == e:
                    continue
                self._wait(e, ev)
        self.last_w = {}
        self.readers = {}

    def finish(self):
        for q, ring in self.dma_ring.items():
            for ent in ring:
                if ent[1] > 0:
                    self._wait("sp", (ent[0], ent[1] * 16, id(ent[0]), "dma"))
        for e in self.eng:
            if e != "sp" and self.cnt[e] > 0:
                self._wait("sp", (self.sem[e], self.cnt[e], id(self.sem[e]), e))

    def sb(self, name, shape, dtype, stack=None):
        self.uid += 1
        nm = f"{name}_{self.uid}"
        h = (stack or self.stack).enter_context(self.nc.sbuf_tensor(nm, list(shape), dtype))
        return Tl(h, nm)

    def ps(self, name, shape, dtype, stack=None):
        self.uid += 1
        nm = f"{name}_{self.uid}"
        h = (stack or self.stack).enter_context(self.nc.psum_tensor(nm, list(shape), dtype))
        return Tl(h, nm, psum=True)

    def dram(self, name, shape, dtype, kind="Internal"):
        h = self.nc.dram_tensor(name, list(shape), dtype, kind=kind)
        return Tl(h.ap(), name)

    @contextlib.contextmanager
    def scope(self):
        st = contextlib.ExitStack()
        try:
            yield st
        finally:
            self.barrier()
            st.close()

    @staticmethod
    def _a(x):
        return x.ap if isinstance(x, V) else x

    def mm(self, out, lhsT, rhs, start=True, stop=True):
        return self.issue("pe", lambda: self.nc.tensor.matmul(out.ap, lhsT=lhsT.ap, rhs=rhs.ap, start=start, stop=stop),
                          [lhsT, rhs], [out])

    def tr(self, out, in_, ident):
        return self.issue("pe", lambda: self.nc.tensor.transpose(out.ap, in_.ap, ident.ap), [in_, ident], [out])

    def act(self, out, in_, func, bias=None, scale=None, accum=None, e="act"):
        kw = {}
        if bias is not None:
            kw["bias"] = self._a(bias)
        if scale is not None:
            kw["scale"] = self._a(scale)
        if accum is not None:
            kw["accum_out"] = accum.ap
        return self.issue(e, lambda: self.nc.scalar.activation(out=out.ap, in_=in_.ap, func=func, **kw),
                          [in_, bias, scale], [out, accum])

    def ts(self, e, out, in0, s1, op0, s2=None, op1=None, accum=None):
        kw = {}
        if op1 is not None:
            kw["op1"] = op1
        if accum is not None:
            kw["accum_out"] = accum.ap
        return self.issue(e, lambda: self.eng[e].tensor_scalar(out=out.ap, in0=in0.ap, scalar1=self._a(s1),
                                                               scalar2=self._a(s2), op0=op0, **kw),
                          [in0, s1, s2], [out, accum])

    def tt(self, e, out, in0, in1, op):
        return self.issue(e, lambda: self.eng[e].tensor_tensor(out=out.ap, in0=in0.ap, in1=in1.ap, op=op),
                          [in0, in1], [out])

    def stt(self, e, out, in0, scalar, in1, op0, op1):
        return self.issue(e, lambda: self.eng[e].scalar_tensor_tensor(out=out.ap, in0=in0.ap, scalar=self._a(scalar),
                                                                      in1=in1.ap, op0=op0, op1=op1),
                          [in0, scalar, in1], [out])

    def copy(self, e, out, in_):
        if e == "act":
            return self.issue(e, lambda: self.nc.scalar.copy(out=out.ap, in_=in_.ap), [in_], [out])
        return self.issue(e, lambda: self.eng[e].tensor_copy(out=out.ap, in_=in_.ap), [in_], [out])

    def memset(self, e, out, val):
        return self.issue(e, lambda: self.eng[e].memset(out.ap, val), [], [out])

    def reduce(self, e, out, in_, op, axis=AX.X):
        return self.issue(e, lambda: self.eng[e].tensor_reduce(out=out.ap, in_=in_.ap, axis=axis, op=op), [in_], [out])

    def recip(self, out, in_):
        return self.issue("dve", lambda: self.nc.vector.reciprocal(out=out.ap, in_=in_.ap), [in_], [out])


D = 4096
KC = D // 128
HD = 128
GDN_H, ATT_H, ATT_KV, HG_H = 12, 12, 4, 8
GDN_W, ATT_W, KV_W, HG_W = 1536, 1536, 512, 1024
N_IN = 13872
QKV_W = 3 * GDN_W
O_Z = 0
O_A = O_Z + GDN_W
O_B = O_A + 24
O_AQ = O_B + 24
O_AK = O_AQ + ATT_W
O_AV = O_AK + KV_W
O_HQ = O_AV + KV_W
O_HF = O_HQ + HG_W
O_HB = O_HF + HG_W
O_HI = O_HB + HG_W
O_HG = O_HI + HG_W
PW = N_IN - QKV_W
NEXP, EFF = 32, 512
EPS = 1e-6


class Cfg:
    def __init__(self, n_ctx_tiles=2, n_lat_tiles=16, depth=2):
        self.nct = n_ctx_tiles
        self.nlt = n_lat_tiles
        self.nt = n_ctx_tiles + n_lat_tiles
        self.T = self.nt * 128
        self.TC = n_ctx_tiles * 128
        self.TL = n_lat_tiles * 128
        self.depth = depth


def phase_ada(k, cfg, g, l):
    nc = k.nc
    with k.scope() as st:
        wts = [k.sb(f"adaw{i}", [128, KC, 256], F32, st) for i in range(2)]
        pss_ = [k.ps(f"adaps{i}", [128, 512], F32, st) for i in range(2)]
        for cb in range(96):
            wt = wts[cb % 2]
            for q4 in range(4):
                wa = g.w[f"ada_{l}_{q4 // 2}"]
                src = wa.h[:, cb * 256:(cb + 1) * 256].rearrange("(c p) n -> p c n", p=128)
                k.dma("sp", wt.sub(q4)[:, q4 * 8:(q4 + 1) * 8, :], wa.v(src[:, (q4 % 2) * 8:(q4 % 2 + 1) * 8, :]))
            for fc in range(2):
                fg = cb * 2 + fc
                pv = pss_[fg % 2][:, 0:2]
                for ch in range(KC):
                    k.mm(pv, wt.sub(ch // 8)[:, ch, fc * 128:(fc + 1) * 128], g.sv[:, ch, :], start=(ch == 0), stop=(ch == KC - 1))
                k.ts("dve", g.modc.sub(l)[:, l, :, fg], pv, g.adab[:, l, fg:fg + 1], ALU.add)
        for s in range(2):
            k.stt("dve", g.g1s.sub(l)[:, l, s, :], g.modc.sub(l)[:, l, s, 32:64], 1.0, g.n1g[:, l, :], ALU.add, ALU.mult)
            k.stt("dve", g.g2s.sub(l)[:, l, s, :], g.modc.sub(l)[:, l, s, 128:160], 1.0, g.n2g[:, l, :], ALU.add, ALU.mult)


def bcast_row(k, g, out_tl, col_v_fn, st_scope):
    dg = k.sb("diag", [128, 128], F32, st_scope)
    ps = k.ps("bcps", [128, 512], F32, st_scope)
    for ch in range(KC):
        k.ts("dve", dg[:], g.identf[:], col_v_fn(ch), ALU.mult)
        k.mm(ps[:, (ch % 4) * 128:(ch % 4 + 1) * 128], g.onesf[:], dg[:])
        if ch % 4 == 3:
            k.copy("act", out_tl[:, (ch - 3) * 128:(ch + 1) * 128], ps[:])


def norm_tile_to_hT(k, g, xt, hT, tcol, gcol_fn, scol_fn, tmp):
    xb, stt_, pst = tmp["xb"], tmp["st"], tmp["pst"]
    k.memset("dve", stt_[:, 0:1], 0.0)
    k.act(xb[:], xt[:], AF.Square, accum=stt_[:, 0:1])
    k.ts("dve", stt_[:, 1:2], stt_[:, 0:1], 1.0 / D, ALU.mult, EPS, ALU.add)
    k.act(stt_[:, 2:3], stt_[:, 1:2], AF.Sqrt)
    k.recip(stt_[:, 3:4], stt_[:, 2:3])
    k.ts("dve", xb[:], xt[:], stt_[:, 3:4], ALU.mult)
    for ch in range(KC):
        p = pst[ch % 2]
        k.tr(p[:], xb[:, ch * 128:(ch + 1) * 128], g.identb[:])
        k.ts("dve", hT.sub(ch)[:, ch, tcol:tcol + 128], p[:], gcol_fn(ch), ALU.mult, scol_fn(ch), ALU.add)


def phase_proj(k, cfg, g, l, xsrc):
    groups = []
    ntg = 9 if cfg.nt > 9 else cfg.nt
    t0 = 0
    while t0 < cfg.nt:
        groups.append((t0, min(ntg, cfg.nt - t0)))
        t0 += ntg
    with k.scope() as st:
        maxn = max(n for _, n in groups) * 128
        hT = k.sb("hT", [128, KC, maxn], BF16, st)
        xts = [k.sb(f"xt{i}", [128, D], F32, st) for i in range(2)]
        tmp = {"xb": k.sb("xb", [128, D], BF16, st), "st": k.sb("nst", [128, 4], F32, st),
               "pst": [k.ps(f"pst{i}", [128, 128], BF16, st) for i in range(2)]}
        wbs = [k.sb(f"wb{i}", [128, KC, 512], BF16, st) for i in range(2)]
        ots = [k.sb(f"ot{i}", [128, 512], F32, st) for i in range(4)]
        pss = [k.ps(f"pps{i}", [128, 512], F32, st) for i in range(4)]
        nblk = (N_IN + 511) // 512
        oi = 0
        for (tg0, ntile) in groups:
            ntok = ntile * 128
            for ti in range(ntile):
                tt = tg0 + ti
                s = 1 if tt < cfg.nct else 0
                xt = xts[ti % 2]
                k.dma("sp", xt[:], xsrc.sub(tt)[tt * 128:(tt + 1) * 128, :])
                norm_tile_to_hT(k, g, xt, hT, ti * 128,
                                lambda ch, s=s: g.g1s.sub(l)[:, l, s, ch:ch + 1],
                                lambda ch, s=s: g.modc.sub(l)[:, l, s, ch:ch + 1], tmp)
            for nb in range(nblk if not DBG.get("skip_mm") else 0):
                c0 = nb * 512
                cw = min(512, N_IN - c0)
                wb = wbs[nb % 2]
                wa = g.w[f"win_{l}"]
                src = wa.h[:, c0:c0 + cw].rearrange("(c p) n -> p c n", p=128)
                for q4 in range(4):
                    k.dma("pool", wb.sub(q4)[:, q4 * 8:(q4 + 1) * 8, 0:cw], wa.v(src[:, q4 * 8:(q4 + 1) * 8, :]))
                if c0 < QKV_W:
                    for sb4 in range(4):
                        col = c0 + sb4 * 128
                        tk = 0
                        while tk < ntok:
                            tw = min(512, ntok - tk)
                            ps = pss[oi % 4]
                            ot = ots[oi % 4]
                            for ch in range(KC):
                                k.mm(ps[:, 0:tw], wb.sub(ch // 8)[:, ch, sb4 * 128:(sb4 + 1) * 128],
                                     hT.sub(ch)[:, ch, tk:tk + tw], start=(ch == 0), stop=(ch == KC - 1))
                            if oi % 2 == 0:
                                k.copy("dve", ot[:, 0:tw], ps[:, 0:tw])
                            else:
                                k.copy("act", ot[:, 0:tw], ps[:, 0:tw])
                            k.dma("sp", g.projT.sub((col // 128, (tg0 * 128 + tk) // 128))[col:col + 128, tg0 * 128 + tk:tg0 * 128 + tk + tw],
                                  ot[:, 0:tw])
                            oi += 1
                            tk += tw
                else:
                    for ti in range(ntile):
                        tt = tg0 + ti
                        ps = pss[oi % 4]
                        ot = ots[oi % 4]
                        for ch in range(KC):
                            k.mm(ps[:, 0:cw], hT.sub(ch)[:, ch, ti * 128:(ti + 1) * 128], wb.sub(ch // 8)[:, ch, 0:cw],
                                 start=(ch == 0), stop=(ch == KC - 1))
                        if oi % 2 == 0:
                            k.copy("dve", ot[:, 0:cw], ps[:, 0:cw])
                        else:
                            k.copy("act", ot[:, 0:cw], ps[:, 0:cw])
                        k.dma("sp", g.proj.sub((tt, nb))[tt * 128:(tt + 1) * 128, c0 - QKV_W:c0 - QKV_W + cw], ot[:, 0:cw])
                        oi += 1


class G:
    pass


def weight_specs(L):
    sp = {}
    for l in range(L):
        for hf in range(2):
            sp[f"ada_{l}_{hf}"] = (2048, 6 * D, 16)
        sp[f"win_{l}"] = (D, N_IN, 32)
        sp[f"wout_{l}"] = (D, D, 128)
        for hf in range(2):
            sp[f"wg_{l}_{hf}"] = (16 * D, EFF, 1024)
            sp[f"wu_{l}_{hf}"] = (16 * D, EFF, 1024)
            sp[f"wd_{l}_{hf}"] = (16 * EFF, D, 128)
    return sp


def weight_full(inputs, name):
    p = name.split("_")
    l = int(p[1])
    if p[0] == "ada":
        hf = int(p[2])
        return inputs["ada_w"][l][hf * 2048:(hf + 1) * 2048]
    if p[0] == "win":
        return inputs["w_in"][l]
    if p[0] == "wout":
        return inputs["w_out"][l]
    hf = int(p[2])
    key = {"wg": "moe_w_gate", "wu": "moe_w_up", "wd": "moe_w_down"}[p[0]]
    w = inputs[key][l][hf * 16:(hf + 1) * 16]
    return w.reshape(-1, w.shape[-1])


def declare(k, cfg, dbg=(), gather=False, use=None, ext=()):
    g = G()
    T = cfg.T
    L = cfg.depth
    g.cfg = cfg

    def din(name, shape, dt=F32):
        return k.dram(name, shape, dt, kind="ExternalInput")

    def scr(name, shape, dt=F32):
        return k.dram(name, shape, dt, kind=("ExternalInput" if name in ext else "ExternalOutput" if name in dbg else "Internal"))

    g.xin = din("xin", [T, D])
    g.cvec = din("cvec", [128, KC, 2])
    g.adab_d = din("adab", [128, L, 192])
    g.n1g_d = din("n1g", [128, L, KC])
    g.n2g_d = din("n2g", [128, L, KC])
    g.identf_d = din("identf", [128, 128])
    g.identb_d = din("identb", [128, 128], BF16)
    g.cst_d = din("cst", [128, 8, 128])
    g.rope_d = din("rope", [128, max(cfg.nlt, 1), 2, 128])
    g.gains_d = din("gains", [128, L, 4, 128])
    g.lbl_d = din("lbl", [128, 2, HG_W])
    g.dtb_d = din("dtb", [128, L, 2, 24])
    g.convw_d = din("convw", [128, L, 36, 5])
    g.rw_d = din("rw", [128, L, KC, 36])
    g.rb_d = din("rb", [128, L, 36])
    g.fng_d = din("fng", [128, D])
    g.w = {}
    g.wsh = {}
    for name, (R, Cc, rp) in weight_specs(L).items():
        if use is not None and name.split("_")[0] not in use:
            continue
        if gather:
            g.wsh[name] = din("sh_" + name, [R // 8, Cc])
            g.w[name] = k.dram("wf_" + name, [R, Cc], F32)
        else:
            g.w[name] = din("wf_" + name, [R, Cc])
    g.projT = scr("projT", [QKV_W, T])
    g.proj = scr("proj", [T, PW])
    g.ymix = scr("ymix", [T, D])
    g.xmid = scr("xmid", [T, D])
    g.xnext = scr("xnext", [T, D])
    g.modc_d = scr("modc_d", [128, L * 2 * 192])
    g.out = k.dram("out", [cfg.TL, D], F32, kind="ExternalOutput")
    g.identf = k.sb("identf", [128, 128], F32)
    g.identb = k.sb("identb", [128, 128], BF16)
    g.onesf = k.sb("onesf", [128, 128], F32)
    g.sv = k.sb("sv", [128, KC, 2], F32)
    g.modc = k.sb("modc", [128, L, 2, 192], F32)
    g.g1s = k.sb("g1s", [128, L, 2, KC], F32)
    g.g2s = k.sb("g2s", [128, L, 2, KC], F32)
    g.adab = k.sb("adab", [128, L, 192], F32)
    g.n1g = k.sb("n1g", [128, L, KC], F32)
    g.n2g = k.sb("n2g", [128, L, KC], F32)
    g.cst = k.sb("cst", [128, 8, 128], F32)
    g.gains = k.sb("gains", [128, L, 4, 128], F32)
    g.dtb = k.sb("dtb", [128, L, 2, 24], F32)
    g.convw = k.sb("convw", [128, L, 36, 5], F32)
    for (t_, d_) in ((g.identf, g.identf_d), (g.identb, g.identb_d), (g.sv, g.cvec), (g.adab, g.adab_d), (g.n1g, g.n1g_d),
                     (g.n2g, g.n2g_d), (g.cst, g.cst_d), (g.gains, g.gains_d), (g.dtb, g.dtb_d), (g.convw, g.convw_d)):
        k.dma("sp", t_[:], V(d_.h, (d_.key,)))
    k.memset("dve", g.onesf[:], 1.0)
    k.act(g.sv[:], g.sv[:], AF.Silu)
    k.act(g.dtb[:, :, 1, :], g.dtb[:, :, 1, :], AF.Exp)
    k.ts("dve", g.dtb[:, :, 1, :], g.dtb[:, :, 1, :], -1.0, ALU.mult)
    g.m32 = [g.cst.v(g.cst.h[0:32, 0, 0:32]), g.cst.v(g.cst.h[0:32, 1, 0:32])]
    g.m64 = [g.cst.v(g.cst.h[0:64, 2, 0:64]), g.cst.v(g.cst.h[0:64, 3, 0:64])]
    g.neg64 = [g.cst.v(g.cst.h[0:64, 4, 0:64]), g.cst.v(g.cst.h[0:64, 5, 0:64])]
    g.offd = g.cst.v(g.cst.h[0:64, 6, 0:64])
    for v_ in g.m32 + g.m64 + g.neg64 + [g.offd]:
        v_.__class__ = VS
    g.qg_bc = Tl(g.gains.h[:, :, 0, :], g.gains.key)
    g.kg_bc = Tl(g.gains.h[:, :, 1, :], g.gains.key)
    g.hgn_bc = Tl(g.gains.h[:, :, 2, :], g.gains.key)
    g.gdn_bc = Tl(g.gains.h[:, :, 3, :], g.gains.key)
    g.dtb_bc = Tl(g.dtb.h[:, :, 0, :], g.dtb.key)
    g.nea_bc = Tl(g.dtb.h[:, :, 1, :], g.dtb.key)
    return g


class VS(V):
    __slots__ = ()

    def __getitem__(self, idx):
        return V(self.ap[idx], self.keys)


def phase_gather(k, cfg, g):
    nc = k.nc
    n = 0
    cc_sem = nc.alloc_semaphore("cc_sem")
    k.all_sems.append(cc_sem)
    for name, (R, Cc, rp) in weight_specs(cfg.depth).items():
        if name not in g.wsh:
            continue
        P = R // (8 * rp)
        sh = g.wsh[name]
        full = g.w[name]
        ib = k.dram("ib_" + name, [R // 8, Cc], F32)
        nel = (R // 8) * Cc
        flat_s = sh.h.rearrange("r c -> (r c)").rearrange("(a b) -> a b", b=2048)
        flat_i = ib.h.rearrange("r c -> (r c)").rearrange("(a b) -> a b", b=2048)
        rows = nel // 2048
        r0 = 0
        while r0 < rows:
            rr = min(1024, rows - r0)
            k.dma("sp", V(flat_i[r0:r0 + rr, :], (ib.key,)), V(flat_s[r0:r0 + rr, :], (sh.key,)))
            r0 += rr
        for p in range(P):
            iv = V(ib.h[p * rp:(p + 1) * rp, :], (ib.key,))
            ov = V(full.h[p * 8 * rp:(p + 1) * 8 * rp, :], (full.key,))
            k._deps("pool", k._keys([iv]), k._keys([ov]))
            nc.gpsimd.collective_compute("AllGather", ALU.bypass, replica_groups=[list(range(8))],
                                         ins=[iv.ap], outs=[ov.ap]).then_inc(cc_sem)
            n += 1
            nc.gpsimd.wait_ge(cc_sem, n)
    k.memset("pool", g.onesf[:, 0:1], 1.0)
    k.barrier()
    return n


def host_prep(inputs, cfg, b, gather, use=None):
    f = np.float32
    L = cfg.depth
    m = {}
    m["xin"] = np.ascontiguousarray(np.concatenate([inputs["ctx"][b][:cfg.TC], inputs["x"][b][:cfg.TL]], axis=0), dtype=f)
    cv = np.stack([inputs["c"][b], inputs["c_ctx"]], axis=-1)
    m["cvec"] = np.ascontiguousarray(cv.reshape(KC, 128, 2).transpose(1, 0, 2), dtype=f)
    m["adab"] = np.ascontiguousarray(inputs["ada_b"][:L].reshape(L, 192, 128).transpose(2, 0, 1), dtype=f)
    m["n1g"] = np.ascontiguousarray(inputs["norm1_g"][:L].reshape(L, KC, 128).transpose(2, 0, 1), dtype=f)
    m["n2g"] = np.ascontiguousarray(inputs["norm2_g"][:L].reshape(L, KC, 128).transpose(2, 0, 1), dtype=f)
    m["identf"] = np.eye(128, dtype=f)
    m["identb"] = np.eye(128, dtype=f).astype(ml_dtypes.bfloat16)
    cst = np.zeros((128, 8, 128), f)
    for i, C in ((0, 32), (2, 64)):
        idx = np.arange(C)
        cst[:C, i, :C] = (idx[:, None] <= idx[None, :])
        cst[:C, i + 1, :C] = (idx[:, None] >= idx[None, :])
    idx = np.arange(64)
    cst[:64, 4, :64] = np.where(idx[None, :] >= idx[:, None], 0.0, -30000.0)
    cst[:64, 5, :64] = np.where(idx[None, :] <= idx[:, None], 0.0, -30000.0)
    cst[:64, 6, :64] = 1.0 - np.eye(64)
    m["cst"] = cst
    nl = cfg.TL
    rows = nl // 64
    row = np.repeat(np.arange(rows, dtype=f), 64)
    col = np.tile(np.arange(64, dtype=f), rows)
    inv = (10000.0 ** (-np.arange(0, 64, 2, dtype=f) / 64)).astype(f)
    ang = np.concatenate([row[:, None] * inv, row[:, None] * inv, col[:, None] * inv, col[:, None] * inv], axis=-1).astype(f)
    cos, sin = np.cos(ang).astype(f), np.sin(ang).astype(f)
    sgn = np.concatenate([-np.ones(32, f), np.ones(32, f), -np.ones(32, f), np.ones(32, f)])
    rope = np.stack([cos, sin * sgn], axis=1)
    m["rope"] = np.ascontiguousarray(rope.reshape(max(cfg.nlt, 1), 128, 2, 128).transpose(1, 0, 2, 3), dtype=f)
    gains = np.stack([inputs["attn_q_norm_g"][:L], inputs["attn_k_norm_g"][:L], inputs["hgrn_norm_g"][:L], inputs["gdn_norm_g"][:L]], axis=1)
    m["gains"] = np.ascontiguousarray(np.broadcast_to(gains[None], (128, L, 4, 128)), dtype=f)
    m["lbl"] = np.ascontiguousarray(np.broadcast_to(inputs["hgrn_lb_logits"][None, :2], (128, 2, HG_W)), dtype=f)
    dtb = np.stack([inputs["gdn_dt_bias"][:L].reshape(L, 24), inputs["gdn_a_log"][:L].reshape(L, 24)], axis=1)
    m["dtb"] = np.ascontiguousarray(np.broadcast_to(dtb[None], (128, L, 2, 24)), dtype=f)
    cw = inputs["gdn_conv_w"][:L]
    m["convw"] = np.ascontiguousarray(cw.reshape(L, 5, 36, 128).transpose(3, 0, 2, 1), dtype=f)
    rw = np.concatenate([inputs["router_group_w"][:L], inputs["router_expert_w"][:L]], axis=-1)
    m["rw"] = np.ascontiguousarray(rw.reshape(L, KC, 128, 36).transpose(2, 0, 1, 3), dtype=f)
    rb = np.concatenate([inputs["router_group_b"][:L], inputs["router_expert_b"][:L]], axis=-1)
    m["rb"] = np.ascontiguousarray(np.broadcast_to(rb[None], (128, L, 36)), dtype=f)
    m["fng"] = np.ascontiguousarray(np.broadcast_to(inputs["final_norm_g"][None], (128, D)), dtype=f)
    for name, (R, Cc, rp) in weight_specs(L).items():
        if use is not None and name.split("_")[0] not in use:
            continue
        W = weight_full(inputs, name)
        if gather:
            P = R // (8 * rp)
            m["sh_" + name] = np.ascontiguousarray(W.reshape(P, 8, rp, Cc)[:, b % 8].reshape(P * rp, Cc), dtype=f)
        else:
            m["wf_" + name] = np.ascontiguousarray(W, dtype=f)
    return m


def build(cfg, gather=True, dbg=(), phases=None, use=None, ext=()):
    nc = bass.Bass("TRN2", target_bir_lowering=False)
    k = KB(nc)
    L = cfg.depth
    with k.stack:
        k.stack.enter_context(nc.allow_low_precision("bf16 matmul operands, fp32 accumulation"))
        g = declare(k, cfg, dbg=dbg, gather=gather, use=use, ext=ext)
        if gather:
            phase_gather(k, cfg, g)
        xs = g.xin
        for l in range(L):
            need_ctx = l < L - 1
            last = l == L - 1
            def on(p):
                return phases is None or (l, p) in phases
            if on("ada"):
                phase_ada(k, cfg, g, l)
            if on("proj"):
                phase_proj(k, cfg, g, l, xs)
            if on("attn"):
                phase_attn(k, cfg, g, l, need_ctx)
            if on("hgrn"):
                phase_hgrn(k, cfg, g, l)
            if on("gdn"):
                phase_gdn(k, cfg, g, l)
            if on("wout"):
                phase_wout(k, cfg, g, l, xs, need_ctx)
            if on("moe"):
                phase_moe(k, cfg, g, l, need_ctx, last)
            xs = g.xnext
        if "modc_d" in dbg:
            k.dma("sp", g.modc_d[:, :], g.modc.v(g.modc.h[:].rearrange("p l s f -> p (l s f)")))
        k.finish()
    return nc, k, g


def kernel(**inputs):
    cfg = Cfg(2, 16, 2)
    inputs = {n: np.asarray(v) for n, v in inputs.items()}
    nc, k, g = build(cfg, gather=True)
    maps = [host_prep(inputs, cfg, b, True) for b in range(8)]
    res = run_bass_kernel_spmd(nc, maps, core_ids=list(range(8)))
    out = np.stack([np.asarray(res.results[b]["out"], dtype=np.float32) for b in range(8)], axis=0)
    return out


def phase_attn(k, cfg, g, l, need_ctx):
    T, NT, NCT = cfg.T, cfg.nt, cfg.nct
    SC = HD ** -0.5
    with k.scope() as st:
        kT = k.sb("kT", [128, ATT_KV, T], BF16, st)
        qT = k.sb("qT", [128, ATT_H, T], BF16, st)
        vA = k.sb("vA", [128, NT, ATT_KV, 132], BF16, st)
        k.memset("pool", vA[:, :, :, 128:129], 1.0)
        with k.scope() as st1:
            rope = k.sb("rope", [128, max(cfg.nlt, 1), 2, 128], F32, st1)
            k.dma("sp", rope[:], V(g.rope_d.h, (g.rope_d.key,)))
            sls = [k.sb(f"sl{i}", [128, 16, 128], F32, st1) for i in range(2)]
            vls = [k.sb(f"vl{i}", [128, 512], F32, st1) for i in range(2)]
            sq = k.sb("sq", [128, 16, 128], F32, st1)
            t2 = k.sb("t2", [128, 16, 128], F32, st1)
            slb = k.sb("slb", [128, 16, 128], BF16, st1)
            stt_ = k.sb("ast", [128, 4, 16], F32, st1)
            pstb = [k.ps(f"apst{i}", [128, 128], BF16, st1) for i in range(2)]
            for tt in range(NT):
                sl = sls[tt % 2]
                vl = vls[tt % 2]
                is_ctx = tt < NCT
                k.dma("sp", sl[:], g.proj.v(g.proj.h[tt * 128:(tt + 1) * 128, O_AQ:O_AQ + 2048].rearrange("p (h d) -> p h d", d=128)))
                k.dma("sp", vl[:], g.proj[tt * 128:(tt + 1) * 128, O_AV:O_AV + 512])
                k.copy("pool", vA[:, tt, :, 0:128], vl.v(vl.h[:].rearrange("p (h d) -> p h d", d=128)))
                k.tt("dve", sq[:], sl[:], sl[:], ALU.mult)
                k.reduce("dve", stt_[:, 0, :], sq[:], ALU.add)
                k.ts("dve", stt_[:, 1, :], stt_[:, 0, :], 1.0 / HD, ALU.mult, EPS, ALU.add)
                k.act(stt_[:, 2, :], stt_[:, 1, :], AF.Sqrt)
                k.recip(stt_[:, 3, :], stt_[:, 2, :])
                k.tt("dve", sl[:], sl[:], stt_.v(stt_.h[:, 3, :].unsqueeze(2).to_broadcast([128, 16, 128])), ALU.mult)
                k.tt("dve", sl[:, 0:12, :], sl[:, 0:12, :], g.qg_bc.v(g.qg_bc.h[:, l, :].unsqueeze(1).to_broadcast([128, 12, 128])), ALU.mult)
                k.tt("dve", sl[:, 12:16, :], sl[:, 12:16, :], g.kg_bc.v(g.kg_bc.h[:, l, :].unsqueeze(1).to_broadcast([128, 4, 128])), ALU.mult)
                if is_ctx:
                    k.copy("dve", slb[:], sl[:])
                else:
                    lt = tt - NCT
                    cosb = rope.v(rope.h[:, lt, 0, :].unsqueeze(1).to_broadcast([128, 16, 128]))
                    for (dst, src) in ((0, 32), (32, 0), (64, 96), (96, 64)):
                        sinb = rope.v(rope.h[:, lt, 1, dst:dst + 32].unsqueeze(1).to_broadcast([128, 16, 32]))
                        k.tt("pool", t2[:, :, dst:dst + 32], sl[:, :, src:src + 32], sinb, ALU.mult)
                    k.tt("dve", sq[:], sl[:], cosb, ALU.mult)
                    k.tt("dve", slb[:], sq[:], t2[:], ALU.add)
                for h in range(16):
                    if is_ctx and h < 12 and not need_ctx:
                        continue
                    p = pstb[h % 2][:]
                    k.tr(p, slb[:, h, :], g.identb[:])
                    if h < 12:
                        k.copy("dve", qT.sub(h)[:, h, tt * 128:(tt + 1) * 128], p)
                    else:
                        k.copy("dve", kT.sub(h - 12)[:, h - 12, tt * 128:(tt + 1) * 128], p)
        with k.scope() as st2:
            pss = [k.ps(f"aps{i}", [128, 512], F32, st2) for i in range(2)]
            pso = [k.ps(f"apo{i}", [128, 132], F32, st2) for i in range(4)]
            pTs = [k.sb(f"pT{i}", [128, 512], BF16, st2) for i in range(3)]
            yacc = [k.sb(f"yacc{i}", [128, 4, ATT_W], F32, st2) for i in range(2)]
            rden = k.sb("rden", [128, 8], F32, st2)
            blocks = []
            if need_ctx:
                blocks.append((0, NCT * 128, 0, NCT))
            q0 = NCT * 128
            while q0 < T:
                qw = min(512, T - q0)
                blocks.append((q0, qw, 0, NT))
                q0 += qw
            it = 0
            for bi, (q0, qw, k0, k1) in enumerate(blocks):
                nqs = qw // 128
                ya = yacc[bi % 2]
                for h in range(ATT_H):
                    kvh = h // 3
                    for kt in range(k0, k1):
                        ps = pss[it % 2]
                        pT = pTs[it % 3]
                        it += 1
                        k.mm(ps[:, 0:qw], kT.sub(kvh)[:, kvh, kt * 128:(kt + 1) * 128], qT.sub(h)[:, h, q0:q0 + qw])
                        k.act(pT[:, 0:qw], ps[:, 0:qw], AF.Exp, scale=SC)
                        for qs in range(nqs):
                            k.mm(pso[qs][:, 0:129], pT[:, qs * 128:(qs + 1) * 128], vA[:, kt, kvh, 0:129],
                                 start=(kt == k0), stop=(kt == k1 - 1))
                    for qs in range(nqs):
                        pv = pso[qs]
                        k.recip(rden[:, qs:qs + 1], pv[:, 128:129])
                        k.ts("dve", ya.sub(qs)[:, qs, h * 128:(h + 1) * 128], pv[:, 0:128], rden[:, qs:qs + 1], ALU.mult)
                for qs in range(nqs):
                    r0 = q0 + qs * 128
                    k.dma("sp", g.ymix.v(g.ymix.h[r0:r0 + 128, GDN_W:GDN_W + ATT_W], ("att", r0)), ya.sub(qs)[:, qs, :])


def seg_list(cfg, csz, nseg_chunks):
    ncc = cfg.TC // csz
    nca = cfg.T // csz
    segs = []
    for (a, b) in ((0, ncc), (ncc, nca)):
        c = a
        while c < b:
            segs.append(list(range(c, min(c + nseg_chunks, b))))
            c += nseg_chunks
    return segs


def phase_hgrn(k, cfg, g, l):
    C = 32
    NCH = cfg.T // C
    SEG = 8
    QS = HD ** -0.5
    segs = seg_list(cfg, C, SEG)
    with k.scope() as st:
        O = k.sb("hO", [C, NCH, 128], F32, st)
        S = k.sb("hS", [128, 128], F32, st)
        Sb = k.sb("hSb", [128, 128], BF16, st)
        ins = {nm: [k.sb(f"h{nm}{i}", [C, SEG, 128], F32, st) for i in range(2)] for nm in ("q", "z", "v")}
        sig = k.sb("hsig", [C, SEG, 128], F32, st)
        lf = k.sb("hlf", [C, SEG, 128], F32, st)
        kk = k.sb("hk", [C, SEG, 128], F32, st)
        gcs = k.sb("hgc", [C, SEG, 128], F32, st)
        e1 = k.sb("he1", [C, SEG, 128], F32, st)
        qd = k.sb("hqd", [C, SEG, 128], BF16, st)
        kd = k.sb("hkd", [C, SEG, 128], BF16, st)
        kr = k.sb("hkr", [C, SEG, 128], BF16, st)
        vb = k.sb("hvb", [C, SEG, 128], BF16, st)
        egt = k.sb("hegt", [128, SEG], F32, st)
        qdT = [k.sb(f"hqdT{i}", [128, C], BF16, st) for i in range(2)]
        kdT = [k.sb(f"hkdT{i}", [128, C], BF16, st) for i in range(2)]
        AT = [k.sb(f"hAT{i}", [C, C], BF16, st) for i in range(2)]
        nst = k.sb("hnst", [C, 4, NCH], F32, st)
        p_gc = [k.ps("hpgc", [128, 512], F32, st)] * 2
        p_gt = [k.ps("hpgt", [128, 512], F32, st)] * 2
        p_trb = [k.ps(f"hptr{i}", [128, 64], BF16, st) for i in range(2)]
        p_m = k.ps("hpm", [128, 512], F32, st)
        p_e = k.ps("hpe", [128, 16], F32, st)
        p_s = k.ps("hps", [128, 512], F32, st)
        ones1 = g.onesf
        lbl = k.sb("hlbl", [C, 2, HG_W], F32, st)
        lbrow = k.sb("hlbrow", [C, 2, HG_W], F32, st)
        omlb = k.sb("homlb", [C, 2, HG_W], F32, st)
        k.dma("sp", lbl[:], g.lbl_d[0:C, :, :])
        k.memset("dve", lbrow[:, 0, :], 0.0)
        k.tt("dve", lbrow[:, 1, :], lbl[:, 1, :], lbl[:, 0, :], ALU.subtract)
        k.act(lbrow[:, 1, :], lbrow[:, 1, :], AF.Sigmoid)
        k.ts("dve", omlb[:], lbrow[:], -1.0, ALU.mult, 1.0, ALU.add)
        ctx_segs = [s_ for s_ in segs if s_[0] * C < cfg.TC]
        lat_segs = [s_ for s_ in segs if s_[0] * C >= cfg.TC]
        for h in range(HG_H):
            for Dr in range(2):
                zoff = O_HF if Dr == 0 else O_HB
                M = g.m32[Dr]
                k.memset("dve", S[:], 0.0)
                k.memset("pool", Sb[:], 0.0)
                if Dr == 0:
                    order = segs
                else:
                    order = [list(reversed(s_)) for s_ in reversed(ctx_segs)] + [list(reversed(s_)) for s_ in reversed(lat_segs)]
                for si, seg in enumerate(order):
                    n = len(seg)
                    c0 = min(seg)
                    r0 = c0 * C
                    q_t, z_t, v_t = ins["q"][si % 2], ins["z"][si % 2], ins["v"][si % 2]
                    for (tl_, off) in ((q_t, O_HQ), (z_t, zoff), (v_t, O_HI)):
                        k.dma("sp", tl_[:, 0:n, :], g.proj.v(g.proj.h[r0:r0 + n * C, off + h * 128:off + (h + 1) * 128].rearrange("(c p) d -> p c d", p=C)))
                    lbr_n = lbrow.v(lbrow.h[:, l, h * 128:(h + 1) * 128].unsqueeze(1).to_broadcast([C, n, 128]))
                    omr_n = omlb.v(omlb.h[:, l, h * 128:(h + 1) * 128].unsqueeze(1).to_broadcast([C, n, 128]))
                    k.act(sig[:, 0:n, :], z_t[:, 0:n, :], AF.Sigmoid)
                    k.tt("dve", lf[:, 0:n, :], sig[:, 0:n, :], omr_n, ALU.mult)
                    k.tt("dve", kk[:, 0:n, :], omr_n, lf[:, 0:n, :], ALU.subtract)
                    k.tt("dve", lf[:, 0:n, :], lf[:, 0:n, :], lbr_n, ALU.add)
                    k.act(lf[:, 0:n, :], lf[:, 0:n, :], AF.Ln)
                    k.act(sig[:, 0:n, :], q_t[:, 0:n, :], AF.Silu)
                    k.copy("pool", vb[:, 0:n, :], v_t[:, 0:n, :])
                    for hf in range((n * 128 + 511) // 512):
                        w = min(512, n * 128 - hf * 512)
                        sl_ = slice(hf * 512, hf * 512 + w)
                        rhs = lf.v(lf.h[:].rearrange("p c d -> p (c d)")[:, sl_])
                        k.mm(p_gc[hf][0:C, 0:w], M[:], rhs)
                        k.mm(p_gt[hf][0:C, 0:w], ones1[0:C, 0:C], rhs)
                        gflat = gcs.v(gcs.h[:].rearrange("p c d -> p (c d)")[:, sl_])
                        e1f = e1.v(e1.h[:].rearrange("p c d -> p (c d)")[:, sl_])
                        k.copy("dve", gflat, p_gc[hf][0:C, 0:w])
                        k.tt("dve", e1f, p_gt[hf][0:C, 0:w], gflat, ALU.subtract)
                    k.act(e1[:, 0:n, :], e1[:, 0:n, :], AF.Exp)
                    k.tt("dve", kr[:, 0:n, :], kk[:, 0:n, :], e1[:, 0:n, :], ALU.mult)
                    k.act(e1[:, 0:n, :], gcs[:, 0:n, :], AF.Exp)
                    k.stt("dve", qd[:, 0:n, :], sig[:, 0:n, :], QS, e1[:, 0:n, :], ALU.mult, ALU.mult)
                    k.act(e1[:, 0:n, :], gcs[:, 0:n, :], AF.Exp, scale=-1.0)
                    k.tt("dve", kd[:, 0:n, :], kk[:, 0:n, :], e1[:, 0:n, :], ALU.mult)
                    for ci in range(n):
                        k.mm(p_e[:, ci:ci + 1], lf[:, ci, :], ones1[0:C, 0:1])
                    k.act(egt[:, 0:n], p_e[:, 0:n], AF.Exp)
                    for c in seg:
                        ci = c - c0
                        pq = p_trb[0][:, 0:C]
                        pk = p_trb[1][:, 0:C]
                        p_a = p_m.sub("a")[0:C, 0:C]
                        p_o = p_m.sub("o")[0:C, 128:256]
                        qT_, kT_, AT_ = qdT[c % 2], kdT[c % 2], AT[c % 2]
                        k.tr(pq, qd[:, ci, :], g.identb[0:C, 0:C])
                        k.tr(pk, kd[:, ci, :], g.identb[0:C, 0:C])
                        k.copy("dve", qT_[:], pq)
                        k.copy("dve", kT_[:], pk)
                        k.mm(p_a, kT_[:], qT_[:])
                        k.tt("dve", AT_[:], p_a, M[:], ALU.mult)
                        k.mm(p_o, qT_[:], Sb[:], start=True, stop=False)
                        k.mm(p_o, AT_[:], vb[:, ci, :], start=False, stop=True)
                        if Dr == 0:
                            k.copy("act", O[:, c, :], p_o)
                        else:
                            k.tt("dve", O[:, c, :], O[:, c, :], p_o, ALU.add)
                        k.mm(p_s[:, 0:128], kr[:, ci, :], vb[:, ci, :])
                        k.stt("dve", S[:], S[:], egt[:, ci:ci + 1], p_s[:, 0:128], ALU.mult, ALU.add)
                        k.copy("act", Sb[:], S[:])
            for si, seg in enumerate(segs):
                n = len(seg)
                c0 = seg[0]
                r0 = c0 * C
                g_t = ins["z"][si % 2]
                k.dma("sp", g_t[:, 0:n, :], g.proj.v(g.proj.h[r0:r0 + n * C, O_HG + h * 128:O_HG + (h + 1) * 128].rearrange("(c p) d -> p c d", p=C)))
                Os = O[:, c0:c0 + n, :]
                k.tt("dve", e1[:, 0:n, :], Os, Os, ALU.mult)
                k.reduce("dve", nst[:, 0, 0:n], e1[:, 0:n, :], ALU.add)
                k.ts("dve", nst[:, 1, 0:n], nst[:, 0, 0:n], 1.0 / HD, ALU.mult, EPS, ALU.add)
                k.act(nst[:, 2, 0:n], nst[:, 1, 0:n], AF.Sqrt)
                k.recip(nst[:, 3, 0:n], nst[:, 2, 0:n])
                k.tt("dve", e1[:, 0:n, :], Os, nst.v(nst.h[:, 3, 0:n].unsqueeze(2).to_broadcast([C, n, 128])), ALU.mult)
                k.tt("dve", e1[:, 0:n, :], e1[:, 0:n, :], g.hgn_bc.v(g.hgn_bc.h[0:C, l, :].unsqueeze(1).to_broadcast([C, n, 128])), ALU.mult)
                k.act(sig[:, 0:n, :], g_t[:, 0:n, :], AF.Silu)
                k.tt("dve", e1[:, 0:n, :], e1[:, 0:n, :], sig[:, 0:n, :], ALU.mult)
                k.dma("sp", g.ymix.v(g.ymix.h[r0:r0 + n * C, GDN_W + ATT_W + h * 128:GDN_W + ATT_W + (h + 1) * 128].rearrange("(c p) d -> p c d", p=C), ("hg", h, si)),
                      e1[:, 0:n, :])


def phase_gdn(k, cfg, g, l):
    C = 64
    T = cfg.T
    NCH = T // C
    NCC = cfg.TC // C
    QS = HD ** -0.5
    with k.scope() as st:
        ab = k.sb("gab", [C, NCH, 128], F32, st)
        gl = k.sb("ggl", [C, NCH, 24], F32, st)
        beta = k.sb("gbeta", [C, NCH, 24], F32, st)
        nbeta = k.sb("gnbeta", [C, NCH, 24], F32, st)
        gl2 = k.sb("ggl2", [C, 2, NCH, 12], F32, st)
        gc = k.sb("ggc", [C, 2, NCH, 12], F32, st)
        egc = k.sb("gegc", [C, 2, NCH, 12], F32, st)
        erem = k.sb("gerem", [C, 2, NCH, 12], F32, st)
        egt = k.sb("gegt", [128, 2, NCH, 12], F32, st)
        k.dma("sp", ab[:], g.proj.v(g.proj.h[:, O_A:O_A + 128].rearrange("(c p) d -> p c d", p=C)))
        k.act(beta[:], ab[:, :, 24:48], AF.Sigmoid)
        k.ts("dve", nbeta[:], beta[:], -1.0, ALU.mult)
        k.tt("dve", gl[:], ab[:, :, 0:24], g.dtb_bc.v(g.dtb_bc.h[0:C, l, :].unsqueeze(1).to_broadcast([C, NCH, 24])), ALU.add)
        k.act(gl[:], gl[:], AF.Exp)
        k.act(gl[:], gl[:], AF.Ln, bias=g.onesf[0:C, 0:1])
        NW = NCH * 12
        with k.scope() as st0:
            pa = k.ps("gpa", [128, 512], F32, st0)
            pb = k.ps("gpb", [128, 512], F32, st0)
            for Dr in range(2):
                k.tt("dve", gl2[:, Dr, :, :], gl[:, :, Dr * 12:(Dr + 1) * 12],
                     g.nea_bc.v(g.nea_bc.h[0:C, l, Dr * 12:(Dr + 1) * 12].unsqueeze(1).to_broadcast([C, NCH, 12])), ALU.mult)
                rhs = gl2.v(gl2.h[:, Dr, :, :].rearrange("p c h -> p (c h)"))
                lvl = DBG.get("gdn_lvl", 9)
                if lvl >= 1:
                    k.mm(pa[0:C, 0:NW], g.m64[Dr][:], rhs)
                if lvl >= 2:
                    k.mm(pb[:, 0:NW], g.onesf[0:C, :], rhs)
                gcf = gc.v(gc.h[:, Dr, :, :].rearrange("p c h -> p (c h)"))
                if lvl >= 3:
                    k.copy("dve", gcf, pa[0:C, 0:NW])
                egf = egt.v(egt.h[:, Dr, :, :].rearrange("p c h -> p (c h)"))
                k.copy("dve", egf, pb[:, 0:NW])
                k.tt("dve", erem.v(erem.h[:, Dr, :, :].rearrange("p c h -> p (c h)")), egt.v(egt.h[0:C, Dr, :, :].rearrange("p c h -> p (c h)")), gcf, ALU.subtract)
                k.act(egf, egf, AF.Exp)
            if DBG.get("gdn_lvl", 9) >= 6:
                k.act(egc.v(egc.h[:].rearrange("p d c h -> p (d c h)")), gc.v(gc.h[:].rearrange("p d c h -> p (d c h)")), AF.Exp)
                k.act(erem.v(erem.h[:].rearrange("p d c h -> p (d c h)")), erem.v(erem.h[:].rearrange("p d c h -> p (d c h)")), AF.Exp)
        xin_ = k.sb("gx", [128, T], F32, st)
        cv = k.sb("gcv", [128, T], F32, st)
        tok = [k.sb(f"gtok{i}", [C, NCH, 128], F32, st) for i in range(3)]
        sq = tok[2]
        nst = k.sb("gnst", [C, 4, 2, NCH], F32, st)
        nb16 = [k.sb(f"gnb{i}", [C, NCH, 128], BF16, st) for i in range(2)]
        Vb = k.sb("gVb", [C, NCH, 128], BF16, st)
        QnT = k.sb("gQnT", [128, T], BF16, st)
        KnT = k.sb("gKnT", [128, T], BF16, st)
        O = k.sb("gO", [C, NCH, 128], F32, st)
        S = k.sb("gS", [128, 128], F32, st)
        Sb = k.sb("gSb", [128, 128], BF16, st)
        gz = [k.sb(f"ggz{i}", [C, 4, 128], F32, st) for i in range(2)]
        wk = {nm: k.sb("gw_" + nm, [C, C], F32, st) for nm in ("diff", "dmt", "dmts", "B", "N", "B2", "N2", "Y", "Y2")}
        egrow = k.sb("gw_egrow", [128, C], F32, st)
        wkb = {nm: k.sb("gwb_" + nm, shp, BF16, st) for nm, shp in (("aqk", [C, C]), ("xt", [C, C]), ("kg", [C, 128]), ("wT", [128, C]),
                                                                    ("qgT", [128, C]), ("kr", [C, 128]), ("vn", [C, 128]))}
        ub = k.sb("gub", [C, 128], F32, st)
        gbc = k.sb("ggbc", [C, 128], F32, st)
        pm = [k.ps(f"gpm{i}", [128, 512], F32, st) for i in range(5)]
        ptr = [k.ps(f"gptr{i}", [128, 64], BF16, st) for i in range(2)]
        cwt = g.convw
        for h in range(GDN_H if DBG.get("gdn_stop", 9) >= 2 else 0):
            for j3 in range(3):
                grp = j3 * 12 + h
                r0 = grp * 128
                k.dma("sp", xin_[:], g.projT[r0:r0 + 128, :])
                x, y = xin_, cv
                k.ts("dve", y[:], x[:], cwt[:, l, grp, 2:3], ALU.mult)
                for (s0, s1) in ((0, cfg.TC), (cfg.TC, T)):
                    for jj in (0, 1, 3, 4):
                        sh = jj - 2
                        lo, hi = max(s0, s0 - sh), min(s1, s1 - sh)
                        k.stt("dve", y[:, lo:hi], x[:, lo + sh:hi + sh], cwt[:, l, grp, jj:jj + 1], y[:, lo:hi], ALU.mult, ALU.add)
                k.act(y[:], y[:], AF.Silu)
                for c in range(NCH):
                    pp = pm[c % 2]
                    k.mm(pp[0:C, 0:128], y[:, c * C:(c + 1) * C], g.identf[:])
                    if c % 2 == 0:
                        k.copy("dve", tok[j3][:, c, :], pp[0:C, 0:128])
                    else:
                        k.copy("act", tok[j3][:, c, :], pp[0:C, 0:128])
            k.copy("pool", Vb[:], tok[2][:])
            for j3 in range(2):
                k.tt("dve", sq[:], tok[j3][:], tok[j3][:], ALU.mult)
                k.reduce("dve", nst[:, 0, j3, :], sq[:], ALU.add)
                k.ts("dve", nst[:, 1, j3, :], nst[:, 0, j3, :], EPS, ALU.add)
                k.act(nst[:, 2, j3, :], nst[:, 1, j3, :], AF.Sqrt)
                k.recip(nst[:, 3, j3, :], nst[:, 2, j3, :])
                rb_ = nst.v(nst.h[:, 3, j3, :].unsqueeze(2).to_broadcast([C, NCH, 128]))
                if j3 == 0:
                    k.stt("dve", nb16[0][:], tok[0][:], QS, rb_, ALU.mult, ALU.mult)
                else:
                    k.tt("dve", nb16[1][:], tok[1][:], rb_, ALU.mult)
            for c in range(NCH):
                for j3, dst in ((0, QnT), (1, KnT)):
                    pv = ptr[j3][:, 0:C]
                    k.tr(pv, nb16[j3][:, c, :], g.identb[0:C, 0:C])
                    k.copy("dve", dst[:, c * C:(c + 1) * C], pv)
            for Dr in range(2 if DBG.get("gdn_stop", 9) >= 3 else 0):
                hd = Dr * 12 + h
                MD = g.m64[Dr]
                NEG = g.neg64[Dr]
                k.memset("dve", S[:], 0.0)
                k.memset("pool", Sb[:], 0.0)
                order = list(range(NCH)) if Dr == 0 else (list(range(NCC - 1, -1, -1)) + list(range(NCH - 1, NCC - 1, -1)))
                for c in order:
                    cs = slice(c * C, (c + 1) * C)
                    gcol = gl2[:, Dr, c, h:h + 1]
                    gccol = gc[:, Dr, c, h:h + 1]
                    nbcol = nbeta[:, c, hd:hd + 1]
                    k.ts("dve", gbc[:], g.onesf[0:C, :], gcol, ALU.mult)
                    k.mm(pm[0][:, 0:C], gbc[:], MD[:])
                    ev_ = k.act(egrow[:], pm[0][:, 0:C], AF.Exp)
                    k._wait("dve", ev_)
                    k.stt("dve", wk["diff"][:], pm[0][0:C, 0:C], gccol, NEG[:], ALU.subtract, ALU.add)
                    k.act(wk["dmt"][:], wk["diff"][:], AF.Exp)
                    k.tt("pool", wk["dmts"][:], wk["dmt"][:], g.offd[:], ALU.mult)
                    k.mm(pm[1][0:C, 0:C], KnT[:, cs], KnT[:, cs])
                    k.mm(pm[1][0:C, C:2 * C], KnT[:, cs], QnT[:, cs])
                    k.tt("dve", wkb["aqk"][:], pm[1][0:C, C:2 * C], wk["dmt"][:], ALU.mult)
                    k.stt("dve", wk["B"][:], pm[1][0:C, 0:C], nbcol, wk["dmts"][:], ALU.mult, ALU.mult)
                    k.mm(pm[2][0:C, 0:C], wk["B"][:], g.identf[0:C, 0:C])
                    k.copy("act", wk["N"][:], pm[2][0:C, 0:C])
                    k.tt("pool", wk["Y"][:], wk["B"][:], g.identf[0:C, 0:C], ALU.add)
                    Bc, Nc, Yc = "B", "N", "Y"
                    for m in range(1, 6):
                        Bn, Nn, Yn = ("B2", "N2", "Y2") if Bc == "B" else ("B", "N", "Y")
                        k.mm(pm[2][0:C, 0:C], wk[Bc][:], wk[Nc][:])
                        if m <= 4:
                            k.mm(pm[3][0:C, 0:C], wk[Nc][:], wk[Bc][:])
                        k.copy("act", wk[Nn][:], pm[2][0:C, 0:C])
                        if m <= 4:
                            k.copy("dve", wk[Bn][:], pm[3][0:C, 0:C])
                        k.mm(pm[4][0:C, 0:C], wk[Nn][:], wk[Yc][:])
                        k.tt("dve", wk[Yn][:], pm[4][0:C, 0:C], wk[Yc][:], ALU.add)
                        Bc, Nc, Yc = Bn, Nn, Yn
                    if DBG.get("gdn_stop", 9) == 3:
                        continue
                    k.copy("act", wkb["xt"][:], wk[Yc][:])
                    k.ts("dve", wkb["kg"][:], nb16[1][:, c, :], egc[:, Dr, c, h:h + 1], ALU.mult)
                    k.ts("dve", wkb["kr"][:], nb16[1][:, c, :], erem[:, Dr, c, h:h + 1], ALU.mult)
                    k.mm(pm[2][0:C, 0:128], wkb["xt"][:], Vb[:, c, :])
                    k.mm(pm[3][:, 0:C], wkb["kg"][:], wkb["xt"][:])
                    k.ts("dve", ub[:], pm[2][0:C, 0:128], beta[:, c, hd:hd + 1], ALU.mult)
                    k.copy("act", wkb["wT"][:], pm[3][:, 0:C])
                    k.tt("dve", wkb["qgT"][:], QnT[:, cs], egrow[:], ALU.mult)
                    k.mm(pm[4][0:C, 0:128], wkb["wT"][:], Sb[:])
                    k.stt("dve", wkb["vn"][:], pm[4][0:C, 0:128], nbcol, ub[:], ALU.mult, ALU.add)
                    k.mm(pm[2][0:C, 128:256], wkb["qgT"][:], Sb[:], start=True, stop=False)
                    k.mm(pm[2][0:C, 128:256], wkb["aqk"][:], wkb["vn"][:], start=False, stop=True)
                    if Dr == 0:
                        k.copy("act", O[:, c, :], pm[2][0:C, 128:256])
                    else:
                        k.tt("dve", O[:, c, :], O[:, c, :], pm[2][0:C, 128:256], ALU.add)
                    k.mm(pm[3][:, 128:256], wkb["kr"][:], wkb["vn"][:])
                    k.stt("dve", S[:], S[:], egt[:, Dr, c, h:h + 1], pm[3][:, 128:256], ALU.mult, ALU.add)
                    k.copy("act", Sb[:], S[:])
            for c4 in range(0, NCH, 4):
                n = min(4, NCH - c4)
                zt = gz[(c4 // 4) % 2]
                r0 = c4 * C
                k.dma("sp", zt[:, 0:n, :], g.proj.v(g.proj.h[r0:r0 + n * C, O_Z + h * 128:O_Z + (h + 1) * 128].rearrange("(c p) d -> p c d", p=C)))
                Os = O[:, c4:c4 + n, :]
                k.tt("dve", sq[:, 0:n, :], Os, Os, ALU.mult)
                k.reduce("dve", nst[:, 0, 0, 0:n], sq[:, 0:n, :], ALU.add)
                k.ts("dve", nst[:, 1, 0, 0:n], nst[:, 0, 0, 0:n], 1.0 / HD, ALU.mult, EPS, ALU.add)
                k.act(nst[:, 2, 0, 0:n], nst[:, 1, 0, 0:n], AF.Sqrt)
                k.recip(nst[:, 3, 0, 0:n], nst[:, 2, 0, 0:n])
                k.tt("dve", sq[:, 0:n, :], Os, nst.v(nst.h[:, 3, 0, 0:n].unsqueeze(2).to_broadcast([C, n, 128])), ALU.mult)
                k.tt("dve", sq[:, 0:n, :], sq[:, 0:n, :], g.gdn_bc.v(g.gdn_bc.h[0:C, l, :].unsqueeze(1).to_broadcast([C, n, 128])), ALU.mult)
                k.act(zt[:, 0:n, :], zt[:, 0:n, :], AF.Silu)
                k.tt("dve", sq[:, 0:n, :], sq[:, 0:n, :], zt[:, 0:n, :], ALU.mult)
                k.dma("sp", g.ymix.v(g.ymix.h[r0:r0 + n * C, h * 128:(h + 1) * 128].rearrange("(c p) d -> p c d", p=C), ("gdn", h, c4)), sq[:, 0:n, :])


def tile_groups(tiles, n):
    return [tiles[i:i + n] for i in range(0, len(tiles), n)]


def phase_wout(k, cfg, g, l, xsrc, need_ctx):
    tiles = list(range(cfg.nt)) if need_ctx else list(range(cfg.nct, cfg.nt))
    with k.scope() as st:
        gbc = [k.sb(f"g1bc{s}", [128, D], F32, st) for s in range(2)]
        with k.scope() as st0:
            for s in range(2):
                if s == 1 and not need_ctx:
                    continue
                bcast_row(k, g, gbc[s], lambda ch, s=s: g.modc.sub(l)[:, l, s, 64 + ch:64 + ch + 1], st0)
        yT = k.sb("yT", [128, KC, 6 * 128], BF16, st)
        yts = [k.sb("yt", [128, D], F32, st)] * 2
        yb = k.sb("yb", [128, D], BF16, st)
        pst = [k.ps(f"wpst{i}", [128, 128], BF16, st) for i in range(2)]
        wbs = [k.sb(f"wo{i}", [128, KC, 512], BF16, st) for i in range(2)]
        xcs = [k.sb(f"xc{i}", [128, 512], F32, st) for i in range(3)]
        tms = [k.sb(f"wtm{i}", [128, 512], F32, st) for i in range(2)]
        pss = [k.ps(f"wps{i}", [128, 512], F32, st) for i in range(3)]
        it = 0
        for grp in tile_groups(tiles, 6):
            for ti, tt in enumerate(grp):
                yt = yts[ti % 2]
                k.dma("sp", yt[:], g.ymix[tt * 128:(tt + 1) * 128, :])
                k.copy("pool", yb[:], yt[:])
                for ch in range(KC):
                    pv = pst[ch % 2][:]
                    k.tr(pv, yb[:, ch * 128:(ch + 1) * 128], g.identb[:])
                    k.copy("dve", yT.sub(ch)[:, ch, ti * 128:(ti + 1) * 128], pv)
            for cb in range(8):
                wb = wbs[cb % 2]
                wa = g.w[f"wout_{l}"]
                src = wa.h[:, cb * 512:(cb + 1) * 512].rearrange("(c p) n -> p c n", p=128)
                for q4 in range(4):
                    k.dma("pool", wb.sub(q4)[:, q4 * 8:(q4 + 1) * 8, :], wa.v(src[:, q4 * 8:(q4 + 1) * 8, :]))
                for ti, tt in enumerate(grp):
                    s = 1 if tt < cfg.nct else 0
                    ps = pss[it % 3]
                    xc = xcs[it % 3]
                    tm = tms[it % 2]
                    it += 1
                    k.dma("sp", xc[:], xsrc[tt * 128:(tt + 1) * 128, cb * 512:(cb + 1) * 512])
                    for ch in range(KC):
                        k.mm(ps[:], yT.sub(ch)[:, ch, ti * 128:(ti + 1) * 128], wb.sub(ch // 8)[:, ch, :], start=(ch == 0), stop=(ch == KC - 1))
                    k.tt("dve", tm[:], ps[:], gbc[s][:, cb * 512:(cb + 1) * 512], ALU.mult)
                    k.tt("pool", xc[:], xc[:], tm[:], ALU.add)
                    k.dma("sp", g.xmid.v(g.xmid.h[tt * 128:(tt + 1) * 128, cb * 512:(cb + 1) * 512], ("xm", tt, cb)), xc[:])


def phase_moe(k, cfg, g, l, need_ctx, last):
    GT = 3
    groups = []
    if need_ctx:
        groups += tile_groups(list(range(cfg.nct)), GT)
    groups += tile_groups(list(range(cfg.nct, cfg.nt)), GT)
    with k.scope() as st:
        hT = k.sb("mhT", [128, KC, GT * 128], BF16, st)
        yacc = k.sb("myacc", [128, GT, D], F32, st)
        comb = k.sb("mcomb", [128, GT, NEXP], F32, st)
        wi = 0
        for grp in groups:
            s = 1 if grp[0] < cfg.nct else 0
            with k.scope() as sa:
                hTf = k.sb("mhTf", [128, 4, 128], F32, sa)
                xt = k.sb("mxt", [128, D], F32, sa)
                xb = k.sb("mxb", [128, D], F32, sa)
                nst = k.sb("mnst", [128, 4], F32, sa)
                rl = k.sb("mrl", [128, 64], F32, sa)
                rs = k.sb("mrs", [128, 16, 8], F32, sa)
                ptf = [k.ps(f"mptf{i}", [128, 128], F32, sa) for i in range(2)]
                pr = k.ps("mpr", [128, 512], F32, sa)
                rwt = k.sb("mrw", [128, KC, 36], F32, sa)
                rbt = k.sb("mrb", [128, 36], F32, sa)
                k.dma("sp", rwt[:], g.rw_d[:, l, :, :])
                k.dma("sp", rbt[:], g.rb_d[:, l, :])
                for ti, tt in enumerate(grp):
                    k.dma("sp", xt[:], g.xmid[tt * 128:(tt + 1) * 128, :])
                    k.memset("dve", nst[:, 0:1], 0.0)
                    k.act(xb[:], xt[:], AF.Square, accum=nst[:, 0:1])
                    k.ts("dve", nst[:, 1:2], nst[:, 0:1], 1.0 / D, ALU.mult, EPS, ALU.add)
                    k.act(nst[:, 2:3], nst[:, 1:2], AF.Sqrt)
                    k.recip(nst[:, 3:4], nst[:, 2:3])
                    k.ts("dve", xb[:], xt[:], nst[:, 3:4], ALU.mult)
                    for ch in range(KC):
                        pv = ptf[ch % 2][:]
                        hv = hTf.sub(ch % 4)[:, ch % 4, :]
                        k.mm(pv, xb[:, ch * 128:(ch + 1) * 128], g.identf[:])
                        k.ts("dve", hv, pv, g.g2s.sub(l)[:, l, s, ch:ch + 1], ALU.mult, g.modc.sub(l)[:, l, s, 96 + ch:96 + ch + 1], ALU.add)
                        k.copy("act", hT.sub((ti, ch))[:, ch, ti * 128:(ti + 1) * 128], hv)
                        k.mm(pr[:, 0:36], hv, rwt[:, ch, :], start=(ch == 0), stop=(ch == KC - 1))
                    k.tt("dve", rl[:, 0:36], pr[:, 0:36], rbt[:], ALU.add)
                    k.reduce("dve", rl[:, 40:41], rl[:, 0:4], ALU.max)
                    k.ts("dve", rl[:, 44:48], rl[:, 0:4], rl[:, 40:41], ALU.is_equal)
                    k.ts("dve", rl[:, 48:52], rl[:, 0:4], rl[:, 40:41], ALU.subtract)
                    k.act(rl[:, 48:52], rl[:, 48:52], AF.Exp)
                    k.reduce("dve", rl[:, 41:42], rl[:, 48:52], ALU.add)
                    k.recip(rl[:, 42:43], rl[:, 41:42])
                    k.ts("dve", rs[:, 0, :], rl[:, 4:12], rl[:, 44:45], ALU.mult)
                    for gi in range(1, 4):
                        k.stt("dve", rs[:, 5, :], rl[:, 4 + gi * 8:12 + gi * 8], rl[:, 44 + gi:45 + gi], rs[:, 0, :], ALU.mult, ALU.add)
                        k.copy("dve", rs[:, 0, :], rs[:, 5, :])
                    k.reduce("dve", rl[:, 52:53], rs[:, 0, :], ALU.max)
                    k.ts("dve", rs[:, 1, :], rs[:, 0, :], rl[:, 52:53], ALU.is_equal)
                    k.stt("dve", rs[:, 2, :], rs[:, 1, :], -1.0e30, rs[:, 0, :], ALU.mult, ALU.add)
                    k.reduce("dve", rl[:, 53:54], rs[:, 2, :], ALU.max)
                    k.ts("dve", rs[:, 3, :], rs[:, 2, :], rl[:, 53:54], ALU.is_equal)
                    k.tt("dve", rl[:, 54:55], rl[:, 53:54], rl[:, 52:53], ALU.subtract)
                    k.act(rl[:, 55:56], rl[:, 54:55], AF.Exp)
                    k.ts("dve", rl[:, 56:57], rl[:, 55:56], 1.0, ALU.add)
                    k.recip(rl[:, 57:58], rl[:, 56:57])
                    k.tt("dve", rl[:, 58:59], rl[:, 55:56], rl[:, 57:58], ALU.mult)
                    k.tt("dve", rl[:, 59:60], rl[:, 57:58], rl[:, 42:43], ALU.mult)
                    k.tt("dve", rl[:, 60:61], rl[:, 58:59], rl[:, 42:43], ALU.mult)
                    k.ts("dve", rs[:, 4, :], rs[:, 1, :], rl[:, 59:60], ALU.mult)
                    k.stt("dve", rs[:, 6, :], rs[:, 3, :], rl[:, 60:61], rs[:, 4, :], ALU.mult, ALU.add)
                    for gi in range(4):
                        k.ts("dve", comb[:, ti, gi * 8:(gi + 1) * 8], rs[:, 6, :], rl[:, 44 + gi:45 + gi], ALU.mult)
                    k.memset("pool", yacc[:, ti, :], 0.0)
            with k.scope() as sb_:
                wg = [k.sb(f"mwg{i}", [128, KC, 256], BF16, sb_) for i in range(2)]
                wu = [k.sb(f"mwu{i}", [128, KC, 256], BF16, sb_) for i in range(2)]
                wd = [k.sb(f"mwd{i}", [128, 2, D], BF16, sb_) for i in range(2)]
                he = k.sb("mhe", [128, 256], F32, sb_)
                hs = k.sb("mhs", [128, 256], F32, sb_)
                heb = k.sb("mheb", [128, 256], BF16, sb_)
                heT = k.sb("mheT", [128, 2, 128], BF16, sb_)
                tmb = [k.sb(f"mtmb{i}", [128, 512], F32, sb_) for i in range(2)]
                ptb = [k.ps(f"mptb{i}", [128, 128], BF16, sb_) for i in range(2)]
                pgu = [k.ps(f"mpgu{i}", [128, 512], F32, sb_) for i in range(2)]
                pdn = [k.ps(f"mpdn{i}", [128, 512], F32, sb_) for i in range(4)]
                for e in range(NEXP):
                    for hf in range(2):
                        wgt, wut, wdt = wg[wi % 2], wu[wi % 2], wd[wi % 2]
                        wi += 1
                        wga, wua, wda = g.w[f"wg_{l}_{e // 16}"], g.w[f"wu_{l}_{e // 16}"], g.w[f"wd_{l}_{e // 16}"]
                        e16 = e % 16
                        srcg = wga.h[e16 * D:(e16 + 1) * D, hf * 256:(hf + 1) * 256].rearrange("(c p) n -> p c n", p=128)
                        srcu = wua.h[e16 * D:(e16 + 1) * D, hf * 256:(hf + 1) * 256].rearrange("(c p) n -> p c n", p=128)
                        srcd = wda.h[e16 * EFF + hf * 256:e16 * EFF + (hf + 1) * 256, :].rearrange("(c p) n -> p c n", p=128)
                        for q4 in range(4):
                            k.dma("pool", wgt.sub(q4)[:, q4 * 8:(q4 + 1) * 8, :], wga.v(srcg[:, q4 * 8:(q4 + 1) * 8, :]))
                            k.dma("pool", wut.sub(q4)[:, q4 * 8:(q4 + 1) * 8, :], wua.v(srcu[:, q4 * 8:(q4 + 1) * 8, :]))
                        for fc in range(2):
                            k.dma("pool", wdt.sub(fc)[:, fc, :], wda.v(srcd[:, fc, :]))
                        for ti, tt in enumerate(grp):
                            for ch in range(KC):
                                k.mm(pgu[0][:, 0:256], hT.sub((ti, ch))[:, ch, ti * 128:(ti + 1) * 128], wgt.sub(ch // 8)[:, ch, :], start=(ch == 0), stop=(ch == KC - 1))
                            for ch in range(KC):
                                k.mm(pgu[1][:, 0:256], hT.sub((ti, ch))[:, ch, ti * 128:(ti + 1) * 128], wut.sub(ch // 8)[:, ch, :], start=(ch == 0), stop=(ch == KC - 1))
                            k.act(hs[:], pgu[0][:, 0:256], AF.Silu)
                            k.stt("dve", he[:], pgu[1][:, 0:256], comb[:, ti, e:e + 1], hs[:], ALU.mult, ALU.mult)
                            k.copy("act", heb[:], he[:])
                            for fc in range(2):
                                pv = ptb[fc][:]
                                k.tr(pv, heb[:, fc * 128:(fc + 1) * 128], g.identb[:])
                                k.copy("dve", heT.sub(fc)[:, fc, :], pv)
                            for half in range(2):
                                for cb in range(4):
                                    col = (half * 4 + cb) * 512
                                    yv = yacc.sub((ti, half, cb))[:, ti, col:col + 512]
                                    for fc in range(2):
                                        k.mm(pdn[cb][:], heT.sub(fc)[:, fc, :], wdt.sub(fc)[:, fc, col:col + 512], start=(fc == 0), stop=(fc == 1))
                                    if cb % 2 == 0:
                                        k.tt("dve", yv, yv, pdn[cb][:], ALU.add)
                                    else:
                                        tm = tmb[cb // 2]
                                        k.copy("act", tm[:], pdn[cb][:])
                                        k.tt("pool", yv, yv, tm[:], ALU.add)
            with k.scope() as sc:
                xt = k.sb("mxt2", [128, D], F32, sc)
                xb = k.sb("mxb2", [128, D], F32, sc)
                nst = k.sb("mnst2", [128, 4], F32, sc)
                gbc = k.sb("g2bc", [128, D], F32, sc)
                bcast_row(k, g, gbc, lambda ch, s=s: g.modc.sub(l)[:, l, s, 160 + ch:160 + ch + 1], sc)
                if last:
                    fng = k.sb("mfng", [128, D], F32, sc)
                    k.dma("sp", fng[:], g.fng_d[:, :])
                for ti, tt in enumerate(grp):
                    k.dma("sp", xt[:], g.xmid[tt * 128:(tt + 1) * 128, :])
                    yk = V(yacc.h[:, ti, :], tuple((yacc.key, (ti, half, cb)) for half in range(2) for cb in range(4)) + (yacc.key,))
                    k.tt("dve", xb[:], yk, gbc[:], ALU.mult)
                    k.tt("dve", xt[:], xt[:], xb[:], ALU.add)
                    if not last:
                        k.dma("sp", g.xnext.v(g.xnext.h[tt * 128:(tt + 1) * 128, :], ("xn", tt)), xt[:])
                    else:
                        k.memset("dve", nst[:, 0:1], 0.0)
                        k.act(xb[:], xt[:], AF.Square, accum=nst[:, 0:1])
                        k.ts("dve", nst[:, 1:2], nst[:, 0:1], 1.0 / D, ALU.mult, EPS, ALU.add)
                        k.act(nst[:, 2:3], nst[:, 1:2], AF.Sqrt)
                        k.recip(nst[:, 3:4], nst[:, 2:3])
                        k.stt("dve", xb[:], xt[:], nst[:, 3:4], fng[:], ALU.mult, ALU.mult)
                        lt = tt - cfg.nct
                        k.dma("sp", g.out.v(g.out.h[lt * 128:(lt + 1) * 128, :], ("out", lt)), xb[:])
```
